# Optimizing a Trainium2 kernel written in Bass

```python
import jax, jax.numpy as jnp
from jax import lax
import numpy as np

D_MODEL = 1024
BATCH = 2
SEQ = 16384
DEPTH = 2

SB_HEADS = 4
SB_HEAD_DIM = 64
Q_BLOCK = 128
RET_HEADS = 4
RET_QK_DIM = 64
RET_V_DIM = 128
RET_CHUNK = 128
ROPE_BASE = 10000.0
POOL_GROUPS = 4
POOL_GROUP_DIM = 64
POOL_WINDOWS = (2, 4, 8, 16)
N_BRANCHES = 3
D_FF = 2816
EPS = 1e-6

SB_W = SB_HEADS * SB_HEAD_DIM
RET_QK_W = RET_HEADS * RET_QK_DIM
RET_V_W = RET_HEADS * RET_V_DIM
POOL_W = POOL_GROUPS * POOL_GROUP_DIM
SPLITS = (SB_W, SB_W, SB_W, RET_QK_W, RET_QK_W, RET_V_W, RET_V_W, POOL_W, N_BRANCHES * D_MODEL)
D_IN = SB_W * 3 + RET_QK_W * 2 + RET_V_W * 2 + POOL_W + N_BRANCHES * D_MODEL

kernel_name = "hybrid_sb_retention_pool_macaron"


def rmsnorm(x, g):
    xf = x.astype(jnp.float32)
    y = xf * lax.rsqrt(jnp.mean(xf * xf, axis=-1, keepdims=True) + EPS)
    return (y * g.astype(jnp.float32)).astype(x.dtype)


def swiglu_half(h, w1, w3, w2):
    return 0.5 * ((jax.nn.silu(h @ w1) * (h @ w3)) @ w2)


def stick_breaking_attention(q, k, v):
    B, S, H, dh = q.shape
    qf = (q.astype(jnp.float32) * (dh ** -0.5)).transpose(0, 2, 1, 3)
    kf = k.astype(jnp.float32).transpose(0, 2, 1, 3)
    vf = v.astype(jnp.float32).transpose(0, 2, 1, 3)
    nb = S // Q_BLOCK
    q_blocks = qf.reshape(B, H, nb, Q_BLOCK, dh).transpose(2, 0, 1, 3, 4)
    key_pos = jnp.arange(S)

    def block(args):
        qb, start = args
        z = jnp.einsum('bhqd,bhkd->bhqk', qb, kf)
        q_pos = start + jnp.arange(Q_BLOCK)
        mask = key_pos[None, :] < q_pos[:, None]
        log_beta = jax.nn.log_sigmoid(z)
        log_fail = jnp.where(mask, log_beta - z, 0.0)
        after = lax.cumsum(log_fail, axis=3, reverse=True) - log_fail
        w = jnp.where(mask, jnp.exp(log_beta + after), 0.0)
        return jnp.einsum('bhqk,bhkd->bhqd', w, vf)

    out = lax.map(block, (q_blocks, jnp.arange(nb) * Q_BLOCK))
    return out.transpose(1, 0, 3, 2, 4).reshape(B, S, H * dh)


def rotary(x, cos, sin):
    half = x.shape[-1] // 2
    x1, x2 = x[..., :half], x[..., half:]
    c = cos[None, :, None, :]
    s = sin[None, :, None, :]
    return jnp.concatenate([x1 * c - x2 * s, x2 * c + x1 * s], axis=-1)


def retention(q, k, v, g):
    B, S, H, dk = q.shape
    dv = v.shape[-1]
    half = dk // 2
    pos = jnp.arange(S, dtype=jnp.float32)
    inv_freq = ROPE_BASE ** (-jnp.arange(half, dtype=jnp.float32) / half)
    ang = pos[:, None] * inv_freq[None, :]
    cos, sin = jnp.cos(ang), jnp.sin(ang)
    qf = rotary(q.astype(jnp.float32), cos, sin)
    kf = rotary(k.astype(jnp.float32), cos, sin) * (dk ** -0.5)
    vf = v.astype(jnp.float32)
    log_gamma = jnp.log(1.0 - 2.0 ** (-5.0 - jnp.arange(H, dtype=jnp.float32)))

    C = RET_CHUNK
    N = S // C
    qc = qf.reshape(B, N, C, H, dk)
    kc = kf.reshape(B, N, C, H, dk)
    vc = vf.reshape(B, N, C, H, dv)
    idx = jnp.arange(C, dtype=jnp.float32)
    diff = idx[:, None] - idx[None, :]
    causal = diff >= 0
    decay = jnp.where(causal[None], jnp.exp(jnp.where(causal, diff, 0.0)[None] * log_gamma[:, None, None]), 0.0)
    scores = jnp.einsum('bnihd,bnjhd->bnhij', qc, kc) * decay
    intra = jnp.einsum('bnhij,bnjhe->bnihe', scores, vc)

    k_decay = jnp.exp((C - 1 - idx)[:, None] * log_gamma[None, :])
    kv = jnp.einsum('bnjhd,bnjhe->nbhde', kc * k_decay[:, :, None], vc)
    chunk_decay = jnp.exp(C * log_gamma)[None, :, None, None]

    def step(state, kv_n):
        return chunk_decay * state + kv_n, state

    _, states = lax.scan(step, jnp.zeros((B, H, dk, dv), jnp.float32), kv)
    q_decay = jnp.exp((idx + 1)[:, None] * log_gamma[None, :])
    cross = jnp.einsum('bnihd,nbhde->bnihe', qc * q_decay[:, :, None], states)

    y = (intra + cross).reshape(B, S, H, dv)
    y = y * lax.rsqrt(jnp.mean(y * y, axis=-1, keepdims=True) + EPS)
    return y.reshape(B, S, H * dv) * jax.nn.silu(g.astype(jnp.float32))


def pool_mixer(u, w_pool, pool_scale):
    B, S, _ = u.shape
    uf = u.astype(jnp.float32).reshape(B, S, POOL_GROUPS, POOL_GROUP_DIM)
    cs = jnp.concatenate([jnp.zeros((B, 1, POOL_GROUPS, POOL_GROUP_DIM), jnp.float32),
                          jnp.cumsum(uf, axis=1)], axis=1)
    t = jnp.arange(S)
    win = jnp.array(POOL_WINDOWS)
    lo = jnp.maximum(t[:, None] + 1 - win[None, :], 0)
    count = (t[:, None] + 1 - lo).astype(jnp.float32)
    cs_lo = cs[:, lo, jnp.arange(POOL_GROUPS)[None, :], :]
    mean = (cs[:, 1:] - cs_lo) / count[None, :, :, None]
    y = jnp.einsum('bsgc,gce->bsge', mean - uf, w_pool.astype(jnp.float32))
    return y.reshape(B, S, POOL_W) * pool_scale.astype(jnp.float32)


def setup_inputs(seed: int = 0) -> dict:
    key = jax.random.key(seed)
    ks = jax.random.split(key, 20)

    def w(k, shape, fan_in):
        return jax.random.normal(k, shape, jnp.float32) * (fan_in ** -0.5)

    def gain(k, shape):
        return 1.0 + 0.02 * jax.random.normal(k, shape, jnp.float32)

    return {
        "x": jax.random.normal(ks[0], (BATCH, SEQ, D_MODEL), jnp.float32),
        "g_ffn1": gain(ks[1], (DEPTH, D_MODEL)),
        "w1_ffn1": w(ks[2], (DEPTH, D_MODEL, D_FF), D_MODEL),
        "w3_ffn1": w(ks[3], (DEPTH, D_MODEL, D_FF), D_MODEL),
        "w2_ffn1": w(ks[4], (DEPTH, D_FF, D_MODEL), D_FF),
        "g_mix": gain(ks[5], (DEPTH, D_MODEL)),
        "w_in": w(ks[6], (DEPTH, D_MODEL, D_IN), D_MODEL),
        "w_branch_sb": w(ks[7], (DEPTH, SB_W, D_MODEL), SB_W),
        "w_branch_ret": w(ks[8], (DEPTH, RET_V_W, D_MODEL), RET_V_W),
        "w_branch_pool": w(ks[9], (DEPTH, POOL_W, D_MODEL), POOL_W),
        "w_pool": w(ks[10], (DEPTH, POOL_GROUPS, POOL_GROUP_DIM, POOL_GROUP_DIM), POOL_GROUP_DIM),
        "pool_scale": gain(ks[11], (DEPTH, POOL_W)),
        "w_out": w(ks[12], (DEPTH, D_MODEL, D_MODEL), D_MODEL),
        "g_ffn2": gain(ks[13], (DEPTH, D_MODEL)),
        "w1_ffn2": w(ks[14], (DEPTH, D_MODEL, D_FF), D_MODEL),
        "w3_ffn2": w(ks[15], (DEPTH, D_MODEL, D_FF), D_MODEL),
        "w2_ffn2": w(ks[16], (DEPTH, D_FF, D_MODEL), D_FF),
        "g_final": gain(ks[17], (D_MODEL,)),
    }


def reference(x, g_ffn1, w1_ffn1, w3_ffn1, w2_ffn1, g_mix, w_in, w_branch_sb, w_branch_ret,
              w_branch_pool, w_pool, pool_scale, w_out, g_ffn2, w1_ffn2, w3_ffn2, w2_ffn2, g_final):
    B, S, D = x.shape
    split_points = [int(p) for p in np.cumsum(SPLITS)[:-1]]
    for l in range(DEPTH):
        x = x + swiglu_half(rmsnorm(x, g_ffn1[l]), w1_ffn1[l], w3_ffn1[l], w2_ffn1[l])

        h = rmsnorm(x, g_mix[l])
        proj = h @ w_in[l]
        q_sb, k_sb, v_sb, q_r, k_r, v_r, g_r, u_p, gate = jnp.split(proj, split_points, axis=-1)
        y_sb = stick_breaking_attention(q_sb.reshape(B, S, SB_HEADS, SB_HEAD_DIM),
                                        k_sb.reshape(B, S, SB_HEADS, SB_HEAD_DIM),
                                        v_sb.reshape(B, S, SB_HEADS, SB_HEAD_DIM))
        y_ret = retention(q_r.reshape(B, S, RET_HEADS, RET_QK_DIM),
                          k_r.reshape(B, S, RET_HEADS, RET_QK_DIM),
                          v_r.reshape(B, S, RET_HEADS, RET_V_DIM), g_r)
        y_pool = pool_mixer(u_p, w_pool[l], pool_scale[l])

        gates = jax.nn.sigmoid(gate.astype(jnp.float32)).reshape(B, S, N_BRANCHES, D)
        merged = (gates[:, :, 0] * (y_sb @ w_branch_sb[l].astype(jnp.float32))
                  + gates[:, :, 1] * (y_ret @ w_branch_ret[l].astype(jnp.float32))
                  + gates[:, :, 2] * (y_pool @ w_branch_pool[l].astype(jnp.float32)))
        x = x + merged.astype(x.dtype) @ w_out[l]

        x = x + swiglu_half(rmsnorm(x, g_ffn2[l]), w1_ffn2[l], w3_ffn2[l], w2_ffn2[l])
    return rmsnorm(x, g_final)
```

```python
import numpy as np
import ml_dtypes
import concourse.bass as bass
import concourse.mybir as mybir
from concourse.bass_utils import run_bass_kernel_spmd

F32 = mybir.dt.float32
BF16 = mybir.dt.bfloat16
AF = mybir.ActivationFunctionType
ALU = mybir.AluOpType
NPBF = ml_dtypes.bfloat16

D = 1024
FF = 2816
NK = 8
NF = 22
EPS = 1e-6
TB = 512
NCORES = 8
DEPTH = 2
BATCH = 2
POOL_WINDOWS = (2, 4, 8, 16)

SEM_EPOCH = 30000
N_DMA_SEMS = 6


class Prog:
    ENGS = ("pe", "act", "dve", "pool", "sp")

    def __init__(self, nc):
        self.nc = nc
        self.ops = []
        self.last_w = {}
        self.readers = {}
        self.n_sig = {e: 0 for e in self.ENGS}
        self.n_dma = {e: 0 for e in self.ENGS}
        self.n_cc = 0
        self.last_cc = None

    def op(self, eng, fn, r=(), w=(), dma=False, extra_deps=(), cc=False):
        deps = set(extra_deps)
        for k in r:
            if k in self.last_w:
                deps.add(self.last_w[k])
        for k in w:
            if k in self.last_w:
                deps.add(self.last_w[k])
            for v in self.readers.get(k, {}).values():
                deps.add(v)
        idx = len(self.ops)
        o = dict(eng=eng, fn=fn, deps=deps, dma=dma, idx=idx, cc=cc)
        if cc:
            self.n_cc += 1
            o["ccval"] = self.n_cc
            if self.last_cc is not None:
                deps.add(self.last_cc)
            self.last_cc = idx
        elif dma:
            n = self.n_dma[eng]
            self.n_dma[eng] = n + 1
            o["dslot"] = n % N_DMA_SEMS
            o["dval"] = 16 * (n // N_DMA_SEMS + 1)
            if n >= N_DMA_SEMS:
                o["prev_same_slot"] = True
        else:
            self.n_sig[eng] += 1
            o["sig"] = self.n_sig[eng]
        self.ops.append(o)
        for k in r:
            rk = ("dma", idx) if dma else eng
            self.readers.setdefault(k, {})[rk] = idx
        for k in w:
            self.last_w[k] = idx
            self.readers[k] = {}
        return idx

    def hid(self, e):
        k = id(e)
        if k not in self.hid_cache:
            self.hid_cache[k] = e.partition_id() % 4
        return self.hid_cache[k]

    def barrier(self, skip_cc=False):
        last = {}
        for o in self.ops:
            if o["cc"]:
                if not skip_cc:
                    last[("cc",)] = o["idx"]
            elif o["dma"]:
                last[("dma", o["eng"], o["dslot"])] = o["idx"]
            else:
                last[o["eng"]] = o["idx"]
        deps = set(last.values())
        for e in self.ENGS:
            self.op(e, None, extra_deps=deps)
        self.last_w = {}
        self.readers = {}

    def emit(self):
        nc = self.nc
        n_epochs = {e: (self.n_sig[e] // SEM_EPOCH) + 1 for e in self.ENGS}
        csem = {e: [nc.alloc_semaphore(f"c_{e}_{i}") for i in range(n_epochs[e])] for e in self.ENGS}
        dsem = {e: [nc.alloc_semaphore(f"d_{e}_{i}") for i in range(N_DMA_SEMS)]
                for e in self.ENGS if self.n_dma[e] > 0}
        ccsem = nc.alloc_semaphore("cc_sem") if self.n_cc else None

        def sig_of(o):
            if o["cc"]:
                return (("cc",), ccsem, o["ccval"])
            if o["dma"]:
                return (("d", o["eng"], o["dslot"]), dsem[o["eng"]][o["dslot"]], o["dval"])
            s = o["sig"]
            ep = (s - 1) // SEM_EPOCH
            return (("c", o["eng"], ep), csem[o["eng"]][ep], s - ep * SEM_EPOCH)

        per_eng = {e: [] for e in self.ENGS}
        for o in self.ops:
            per_eng[o["eng"]].append(o)
        ops = self.ops

        def build(ename):
            def body(e):
                self.hid_cache = {}
                waited = {}
                for o in per_eng[ename]:
                    waits = {}
                    for d in o["deps"]:
                        do = ops[d]
                        same = (not do["dma"]) and do["eng"] == ename
                        if same and ename == "pe" and not o["dma"]:
                            continue
                        if same and do["fn"] is None:
                            continue
                        key, sem, val = sig_of(do)
                        if waits.get(key, (None, 0))[1] < val:
                            waits[key] = (sem, val)
                    if o.get("prev_same_slot"):
                        key = ("d", ename, o["dslot"])
                        val = o["dval"] - 16
                        if waits.get(key, (None, 0))[1] < val:
                            waits[key] = (dsem[ename][o["dslot"]], val)
                    for key, (sem, val) in waits.items():
                        if waited.get(key, 0) >= val:
                            continue
                        waited[key] = val
                        e.wait_ge(sem, val)
                    ins = e.nop() if o["fn"] is None else o["fn"](e)
                    key, sem, val = sig_of(o)
                    if o["cc"]:
                        ins.then_inc(sem)
                    elif o["dma"]:
                        ins.then_inc(sem, 16)
                    else:
                        ins.then_inc(sem, 1)
            return body

        with nc.Block() as block:
            block.sync(build("sp"))
            block.tensor(build("pe"))
            block.scalar(build("act"))
            block.vector(build("dve"))
            block.gpsimd(build("pool"))


class Ctx:
    def __init__(self):
        self.nc = bass.Bass("TRN2", target_bir_lowering=False)
        self.P = Prog(self.nc)
        self.base = 16512
        self.top = 229344
        self.off = self.base
        self.uid = 0
        self.pb2 = [self.nc.alloc_psum_tensor(f"pb2_{i}", [128, 1024], F32) for i in range(4)]
        self.pb = [self.pb2[i // 2][:, (i % 2) * 512:(i % 2 + 1) * 512] for i in range(8)]
        self.pbh = [self.pb2[i // 2].bitcast(BF16)[:, (i % 2) * 1024:(i % 2) * 1024 + 1024] for i in range(8)]
        self.soft_reset = False
        self.skip_cc = False

    def reset(self):
        if self.soft_reset:
            self.soft_reset = False
        else:
            self.P.barrier(skip_cc=self.skip_cc)
        self.off = self.base

    def sb(self, name, shape, dtype):
        n = 1
        for s in shape[1:]:
            n *= s
        nbytes = n * (4 if dtype == F32 else 2)
        off = (self.off + 31) // 32 * 32
        assert off + nbytes <= self.top, (name, off, nbytes, self.top)
        self.off = off + nbytes
        self.uid += 1
        return self.nc.alloc_sbuf_tensor_at(f"{name}_{self.uid}", list(shape), dtype, offset=off)

    def din(self, name, shape, dtype):
        return self.nc.dram_tensor(name, list(shape), dtype, kind="ExternalInput")

    def dout(self, name, shape, dtype):
        return self.nc.dram_tensor(name, list(shape), dtype, kind="ExternalOutput")

    def dint(self, name, shape, dtype):
        return self.nc.dram_tensor(name, list(shape), dtype)


def xblk(x_d, b):
    return x_d.ap().rearrange("(k p) t -> p k t", p=128)[:, :, b * TB:(b + 1) * TB]


def emit_consts(C):
    P = C.P
    ones = C.sb("ones", [128, 128], BF16)
    P.op("pool", lambda e: e.memset(ones[:], 1.0), w=["ones"])
    return ones


def emit_rms(C, xb, xkey, ones, gcol, out_fn, sq, rs, stat_bank, tag):
    P = C.P
    pst = C.pb[stat_bank]
    for k in range(NK):
        s = sq[k % 2]
        sk = ("sq", k % 2)
        P.op("pool", lambda e, s=s, k=k: e.tensor_tensor(out=s[:], in0=xb[:, k, :], in1=xb[:, k, :], op=ALU.mult),
             r=[xkey], w=[sk])
        P.op("pe", lambda e, s=s, k=k: e.matmul(pst[:], lhsT=ones[:], rhs=s[:], start=(k == 0), stop=(k == NK - 1)),
             r=[sk, "ones"], w=[("pb", stat_bank)])
    lnv, rstd = rs
    P.op("act", lambda e: e.activation(out=lnv[:], in_=pst[:], func=AF.Ln, bias=EPS, scale=1.0 / D),
         r=[("pb", stat_bank)], w=["lnv"])
    P.op("act", lambda e: e.activation(out=rstd[:], in_=lnv[:], func=AF.Exp, scale=-0.5),
         r=["lnv"], w=["rstd"])
    for k in range(NK):
        oap, okeys = out_fn(k)
        P.op("dve", lambda e, k=k, oap=oap: e.scalar_tensor_tensor(out=oap, in0=xb[:, k, :], scalar=gcol[:, k:k + 1],
                                                                   in1=rstd[:], op0=ALU.mult, op1=ALU.mult),
             r=[xkey, "rstd", "gcol" + tag], w=okeys)


def load_gcol(C, g_d, tag):
    gcol = C.sb("gcol" + tag, [128, NK], F32)
    C.P.op("sp", lambda e: e.dma_start(out=gcol[:], in_=g_d.ap()), w=["gcol" + tag], dma=True)
    return gcol


def load_w_cast(C, dst_ap, src_ap, key):
    C.P.op("pool", lambda e: e.dma_start(out=dst_ap, in_=src_ap, max_dma_last_dim=4096), w=[key], dma=True)


def emit_ffn(C, T, x_src, x_dst, g_d, w1_d, w3_d, w2_d, gfin_d=None):
    P = C.P
    C.reset()
    nb = T // TB
    ones = emit_consts(C)
    gcol = load_gcol(C, g_d, "f")
    gfin = load_gcol(C, gfin_d, "fin") if gfin_d is not None else None
    w1s = C.sb("w1s", [128, NK, FF], BF16)
    w3s = C.sb("w3s", [128, NK, FF], BF16)
    w2s = C.sb("w2s", [128, NF, D], BF16)
    for k in range(NK):
        load_w_cast(C, w1s[:, k, :], w1_d.ap()[k * 128:(k + 1) * 128, :], ("w1", k))
        load_w_cast(C, w3s[:, k, :], w3_d.ap()[k * 128:(k + 1) * 128, :], ("w3", k))
    for f in range(NF):
        load_w_cast(C, w2s[:, f, :], w2_d.ap()[f * 128:(f + 1) * 128, :], ("w2", f))
    xbs = [C.sb("xb", [128, NK, TB], F32) for _ in range(2)]
    hT = C.sb("hT", [128, NK, TB], BF16)
    gT = C.sb("gT", [128, NF, TB], BF16)
    sq = [C.sb("sq", [128, TB], BF16) for _ in range(2)]
    rs = (C.sb("lnv", [128, TB], F32), C.sb("rstd", [128, TB], F32))
    sil = [C.sb("sil", [128, TB], F32) for _ in range(2)]
    w1keys = [("w1", k) for k in range(NK)]
    w3keys = [("w3", k) for k in range(NK)]

    def load_x(b):
        xb = xbs[b % 2]
        P.op("sp", lambda e, xb=xb, b=b: e.dma_start(out=xb[:], in_=xblk(x_src, b)), r=[("xd", b)], w=[("xb", b % 2)], dma=True)

    def do_block(b):
        xb = xbs[b % 2]
        xkey = ("xb", b % 2)
        if b + 1 < nb:
            load_x(b + 1)
        emit_rms(C, xb, xkey, ones, gcol, lambda k: (hT[:, k, :], [("hT", k)]), sq, rs, 0, "f")
        for f in range(NF):
            pa, pbb = C.pb[1 + (f % 2)], C.pb[3 + (f % 2)]
            ka, kb = ("pb", 1 + (f % 2)), ("pb", 3 + (f % 2))
            for k in range(NK):
                P.op("pe", lambda e, k=k, f=f, pa=pa: e.matmul(pa[:], lhsT=w1s[:, k, f * 128:(f + 1) * 128], rhs=hT[:, k, :],
                                                               start=(k == 0), stop=(k == NK - 1)),
                     r=[("hT", k), ("w1", k)], w=[ka])
            for k in range(NK):
                P.op("pe", lambda e, k=k, f=f, pbb=pbb: e.matmul(pbb[:], lhsT=w3s[:, k, f * 128:(f + 1) * 128], rhs=hT[:, k, :],
                                                                 start=(k == 0), stop=(k == NK - 1)),
                     r=[("hT", k), ("w3", k)], w=[kb])
            s = sil[f % 2]
            P.op("act", lambda e, s=s, pa=pa: e.activation(out=s[:], in_=pa[:], func=AF.Silu), r=[ka], w=[("sil", f % 2)])
            P.op("dve", lambda e, s=s, pbb=pbb, f=f: e.tensor_tensor(out=gT[:, f, :], in0=s[:], in1=pbb[:], op=ALU.mult),
                 r=[("sil", f % 2), kb], w=[("gT", f)])
        for c in range(NK):
            py, ky = C.pb[5 + (c % 2)], ("pb", 5 + (c % 2))
            for f in range(NF):
                P.op("pe", lambda e, c=c, f=f, py=py: e.matmul(py[:], lhsT=w2s[:, f, c * 128:(c + 1) * 128], rhs=gT[:, f, :],
                                                               start=(f == 0), stop=(f == NF - 1)),
                     r=[("gT", f), ("w2", f)], w=[ky])
            P.op("dve", lambda e, c=c, py=py: e.scalar_tensor_tensor(out=xb[:, c, :], in0=py[:], scalar=0.5, in1=xb[:, c, :],
                                                                      op0=ALU.mult, op1=ALU.add),
                 r=[ky, xkey], w=[xkey])
        if gfin is not None:
            emit_rms(C, xb, xkey, ones, gfin, lambda k: (xb[:, k, :], [xkey]), sq, rs, 0, "fin")
        P.op("sp", lambda e, b=b, xb=xb: e.dma_start(out=xblk(x_dst, b), in_=xb[:]), r=[xkey], w=[("xd", b)], dma=True)

    load_x(0)
    for b in range(nb):
        do_block(b)


def emit_proj(C, T, x_src, g_d, wq_d, cos_d, sin_d, scv_d, fm_out, tm_out):
    P = C.P
    C.reset()
    nb = T // TB
    ones = emit_consts(C)
    gcol = load_gcol(C, g_d, "p")
    scv = C.sb("scv", [128, 1], F32)
    P.op("sp", lambda e: e.dma_start(out=scv[:], in_=scv_d.ap()), w=["scv"], dma=True)
    wq = C.sb("wq", [128, NK, 3072], BF16)
    for k in range(NK):
        load_w_cast(C, wq[:, k, :], wq_d.ap()[k * 128:(k + 1) * 128, :], ("wq", k))
    xbs = [C.sb("xb", [128, NK, TB], F32) for _ in range(2)]
    hT = C.sb("hT", [128, NK, TB], BF16)
    sq = [C.sb("sq", [128, TB], BF16) for _ in range(2)]
    rs = (C.sb("lnv", [128, TB], F32), C.sb("rstd", [128, TB], F32))
    cs = C.sb("cos", [128, TB], F32)
    sn = C.sb("sin", [128, TB], F32)
    t1 = C.sb("t1", [128, TB], F32)
    t2 = C.sb("t2", [128, TB], F32)
    fms = C.sb("fms", [128, 8, TB], BF16)
    nl = T // 128
    tms = C.sb("tms", [128, 4, 3, nl * 128], BF16)
    hkeys = [("hT", k) for k in range(NK)]
    wkeys = [("wq", k) for k in range(NK)]

    def load_x(b):
        xb = xbs[b % 2]
        P.op("sp", lambda e, xb=xb, b=b: e.dma_start(out=xb[:], in_=xblk(x_src, b)), r=[("xd", b)], w=[("xb", b % 2)], dma=True)

    def do_block(b):
        xb = xbs[b % 2]
        xkey = ("xb", b % 2)
        if b + 1 < nb:
            load_x(b + 1)
        P.op("sp", lambda e, b=b: e.dma_start(out=cs[:], in_=cos_d.ap()[:, b * TB:(b + 1) * TB]), w=["cos"], dma=True)
        P.op("sp", lambda e, b=b: e.dma_start(out=sn[:], in_=sin_d.ap()[:, b * TB:(b + 1) * TB]), w=["sin"], dma=True)
        emit_rms(C, xb, xkey, ones, gcol, lambda k: (hT[:, k, :], [("hT", k)]), sq, rs, 0, "p")

        def fm_mm(j, bank):
            for k in range(NK):
                P.op("pe", lambda e, k=k: e.matmul(C.pb[bank][:], lhsT=wq[:, k, j * 128:(j + 1) * 128], rhs=hT[:, k, :],
                                                   start=(k == 0), stop=(k == NK - 1)),
                     r=[("hT", k), ("wq", k)], w=[("pb", bank)])

        for h in range(4):
            fm_mm(3 * h, 1)
            P.op("dve", lambda e, h=h: e.tensor_scalar(out=fms[:, 2 * h, :], in0=C.pb[1][:], scalar1=scv[:, 0:1], scalar2=None,
                                                       op0=ALU.mult),
                 r=[("pb", 1), "scv"], w=[("fms", 2 * h)])
            fm_mm(3 * h + 1, 2)
            fm_mm(3 * h + 2, 3)
            P.op("dve", lambda e: e.tensor_tensor(out=t1[:], in0=C.pb[2][:], in1=cs[:], op=ALU.mult),
                 r=[("pb", 2), "cos"], w=["t1"])
            P.op("dve", lambda e: e.tensor_tensor(out=t2[:], in0=C.pb[3][:], in1=sn[:], op=ALU.mult),
                 r=[("pb", 3), "sin"], w=["t2"])
            P.op("pool", lambda e, h=h: e.tensor_tensor(out=fms[:, 2 * h + 1, :], in0=t1[:], in1=t2[:], op=ALU.add),
                 r=["t1", "t2"], w=[("fms", 2 * h + 1)])
        P.op("sp", lambda e, b=b: e.dma_start(out=fm_out.ap().rearrange("(j p) t -> p j t", p=128)[:, :, b * TB:(b + 1) * TB], in_=fms[:]),
             r=[("fms", j) for j in range(8)], w=[("fmA", b)], dma=True)
        for s in range(4):
            for j in range(3):
                bank = 4 + j
                for k in range(NK):
                    P.op("pe", lambda e, k=k, s=s, j=j, bank=bank: e.matmul(
                        C.pb[bank][:], lhsT=hT[:, k, s * 128:(s + 1) * 128], rhs=wq[:, k, 1536 + j * 512:1536 + (j + 1) * 512],
                        start=(k == 0), stop=(k == NK - 1)), r=[("hT", k), ("wq", k)], w=[("pb", bank)])
            n_l = 4 * b + s
            P.op("dve", lambda e, n_l=n_l: e.tensor_copy(out=tms[:, :, 0, n_l * 64:(n_l + 1) * 64],
                                                         in_=C.pb[4][:, 0:256].rearrange("p (h d) -> p h d", h=4)),
                 r=[("pb", 4)], w=[("tms", 0)])
            P.op("dve", lambda e, n_l=n_l: e.tensor_copy(out=tms[:, :, 0, nl * 64 + n_l * 64:nl * 64 + (n_l + 1) * 64],
                                                         in_=C.pb[4][:, 256:512].rearrange("p (h d) -> p h d", h=4)),
                 r=[("pb", 4)], w=[("tms", 1)])
            P.op("act", lambda e, n_l=n_l: e.activation(out=tms[:, :, 1, n_l * 128:(n_l + 1) * 128],
                                                        in_=C.pb[5][:].rearrange("p (h d) -> p h d", h=4), func=AF.Copy),
                 r=[("pb", 5)], w=[("tms", 2)])
            P.op("act", lambda e, n_l=n_l: e.activation(out=tms[:, :, 2, n_l * 128:(n_l + 1) * 128],
                                                        in_=C.pb[6][:].rearrange("p (h d) -> p h d", h=4), func=AF.Silu),
                 r=[("pb", 6)], w=[("tms", 3)])

    load_x(0)
    for b in range(nb):
        do_block(b)
    P.op("sp", lambda e: e.dma_start(out=tm_out.ap().rearrange("(h g p) x -> p h g x", h=4, g=3), in_=tms[:]),
         r=[("tms", i) for i in range(4)], w=["tmA"], dma=True)


def emit_merge(C, T, x_src, x_dst, g_d, wg_d, wb_d, wo_d, yG, yL):
    P = C.P
    C.reset()
    nb = T // TB
    ones = emit_consts(C)
    gcol = load_gcol(C, g_d, "m")
    wg = C.sb("wg", [128, NK, 3072], BF16)
    wb = C.sb("wb", [128, NK, D], BF16)
    wo = C.sb("wo", [128, NK, D], BF16)
    for k in range(NK):
        load_w_cast(C, wg[:, k, :], wg_d.ap()[k * 128:(k + 1) * 128, :], ("wg", k))
        load_w_cast(C, wb[:, k, :], wb_d.ap()[k * 128:(k + 1) * 128, :], ("wb", k))
        load_w_cast(C, wo[:, k, :], wo_d.ap()[k * 128:(k + 1) * 128, :], ("wo", k))
    xbs = [C.sb("xb", [128, NK, TB], F32) for _ in range(2)]
    hT = C.sb("hT", [128, NK, TB], BF16)
    yT = C.sb("yT", [128, NK, TB], BF16)
    mT = C.sb("mT", [128, NK, TB], BF16)
    sq = [C.sb("sq", [128, TB], BF16) for _ in range(2)]
    rs = (C.sb("lnv", [128, TB], F32), C.sb("rstd", [128, TB], F32))
    sg = [C.sb("sg", [128, TB], F32) for _ in range(2)]
    tp = [C.sb("tp", [128, TB], F32) for _ in range(3)]
    macc = C.sb("macc", [128, TB], F32)
    branch_k = [(0, 2), (2, 6), (6, 8)]
    for i in range(3):
        P.op(C.dq, lambda e, i=i: e.dma_start(out=yL[i].ap(), in_=yG[i].ap()[:, bass.ds(C.P.hid(e) * T, T)]),
             w=[("yL", i)], dma=True)

    def load_x(b):
        xb = xbs[b % 2]
        P.op("sp", lambda e, xb=xb, b=b: e.dma_start(out=xb[:], in_=xblk(x_src, b)), r=[("xd", b)], w=[("xb", b % 2)], dma=True)

    def do_block(b):
        xb = xbs[b % 2]
        xkey = ("xb", b % 2)
        if b + 1 < nb:
            load_x(b + 1)
        for i, (k0, k1) in enumerate(branch_k):
            P.op("sp", lambda e, b=b, k0=k0, k1=k1, i=i: e.dma_start(
                out=yT[:, k0:k1, :],
                in_=yL[i].ap().rearrange("(k p) t -> p k t", p=128)[:, :, b * TB:(b + 1) * TB]),
                r=[("yL", i)], w=[("yT", k) for k in range(k0, k1)], dma=True)
        emit_rms(C, xb, xkey, ones, gcol, lambda k: (hT[:, k, :], [("hT", k)]), sq, rs, 0, "m")
        it = 0
        for c in range(NK):
            for j in range(3):
                bp, bg = 1 + (it % 2), 3 + (it % 2)
                k0, k1 = branch_k[j]
                for k in range(k0, k1):
                    P.op("pe", lambda e, k=k, c=c, bp=bp, k0=k0, k1=k1: e.matmul(
                        C.pb[bp][:], lhsT=wb[:, k, c * 128:(c + 1) * 128], rhs=yT[:, k, :], start=(k == k0), stop=(k == k1 - 1)),
                        r=[("yT", k), ("wb", k)], w=[("pb", bp)])
                for k in range(NK):
                    P.op("pe", lambda e, k=k, c=c, j=j, bg=bg: e.matmul(
                        C.pb[bg][:], lhsT=wg[:, k, j * D + c * 128:j * D + (c + 1) * 128], rhs=hT[:, k, :],
                        start=(k == 0), stop=(k == NK - 1)), r=[("hT", k), ("wg", k)], w=[("pb", bg)])
                s = sg[it % 2]
                P.op("act", lambda e, s=s, bg=bg: e.activation(out=s[:], in_=C.pb[bg][:], func=AF.Sigmoid),
                     r=[("pb", bg)], w=[("sg", it % 2)])
                P.op("dve", lambda e, s=s, bp=bp, j=j: e.tensor_tensor(out=tp[j][:], in0=s[:], in1=C.pb[bp][:], op=ALU.mult),
                     r=[("sg", it % 2), ("pb", bp)], w=[("tp", j)])
                it += 1
            P.op("pool", lambda e: e.tensor_tensor(out=macc[:], in0=tp[0][:], in1=tp[1][:], op=ALU.add),
                 r=[("tp", 0), ("tp", 1)], w=["macc"])
            P.op("pool", lambda e, c=c: e.tensor_tensor(out=mT[:, c, :], in0=macc[:], in1=tp[2][:], op=ALU.add),
                 r=["macc", ("tp", 2)], w=[("mT", c)])
        for c in range(NK):
            py, ky = C.pb[5 + (c % 2)], ("pb", 5 + (c % 2))
            for k in range(NK):
                P.op("pe", lambda e, c=c, k=k, py=py: e.matmul(py[:], lhsT=wo[:, k, c * 128:(c + 1) * 128], rhs=mT[:, k, :],
                                                               start=(k == 0), stop=(k == NK - 1)),
                     r=[("mT", k), ("wo", k)], w=[ky])
            P.op("dve", lambda e, c=c, py=py: e.tensor_tensor(out=xb[:, c, :], in0=py[:], in1=xb[:, c, :], op=ALU.add),
                 r=[ky, xkey], w=[xkey])
        P.op("sp", lambda e, b=b, xb=xb: e.dma_start(out=xblk(x_dst, b), in_=xb[:]), r=[xkey], w=[("xd", b)], dma=True)

    load_x(0)
    for b in range(nb):
        do_block(b)


def load_fm(C, dst, fmG, S, which, key):
    gv = fmG.ap().rearrange("(j r p) t -> j p r t", r=4, j=8)
    jj, p0 = which // 2, (which % 2) * 64
    C.P.op(C.dq, lambda e: e.dma_start(out=dst[:, :].rearrange("p (r t) -> p r t", r=4),
                                       in_=gv[bass.ds(C.P.hid(e) * 2 + jj, 1), p0:p0 + 64, :, :]),
           r=[("fmG", 2 * h + jj) for h in range(4)], w=[key], dma=True)


def load_tm(C, dst2d, tmG, S, g, off, w, key):
    tv = tmG.ap().rearrange("(h g r p) x -> h g p r x", h=4, g=3, r=4)
    C.P.op(C.dq, lambda e: e.dma_start(out=dst2d[:, :].rearrange("p (r x) -> p r x", r=4),
                                       in_=tv[bass.ds(C.P.hid(e), 1), g, :, :, off:off + w]),
           r=[("tmG", g, h) for h in range(4)], w=[key], dma=True)


def emit_sb(C, S, fmG, tmG, cst_d, ysb_out):
    P = C.P
    C.reset()
    NB = S // 128
    NG = S // TB
    qT = C.sb("qT", [64, S], BF16)
    kT = C.sb("kT", [64, S], BF16)
    V2 = C.sb("V", [128, NB * 64], BF16)
    V = V2[:, :].rearrange("p (n d) -> p n d", d=64)
    tri = C.sb("tri", [128, 128], BF16)
    tric = C.sb("tric", [128, 128], BF16)
    onesm = C.sb("onesm", [128, 128], BF16)
    mk = C.sb("mk", [128, 4, TB], BF16)
    load_fm(C, qT, fmG, S, 0, "qT")
    load_fm(C, kT, fmG, S, 1, "kT")
    load_tm(C, V2, tmG, S, 0, 0, (S // 512) * 64, "V")
    P.op("sp", lambda e: e.dma_start(out=tri[:], in_=cst_d["tri"].ap()), w=["tri"], dma=True)
    P.op("sp", lambda e: e.dma_start(out=tric[:], in_=cst_d["tric"].ap()), w=["tric"], dma=True)
    P.op("sp", lambda e: e.dma_start(out=mk[:], in_=cst_d["mask"].ap().rearrange("p (r t) -> p r t", t=TB)), w=["mk"], dma=True)
    P.op("pool", lambda e: e.memset(onesm[:], 1.0), w=["onesm"])
    E = [C.sb("E", [128, 2, TB], F32) for _ in range(2)]
    L = [C.sb("L", [128, 2, TB], BF16) for _ in range(2)]
    W = [C.sb("W", [128, 2, TB], BF16) for _ in range(2)]
    ost = [C.sb("ost", [64, TB], BF16) for _ in range(2)]
    pz = [C.pb[0], C.pb[1]]
    pA = [C.pb[2], C.pb[3]]
    pX = [C.pb[4], C.pb[5]]
    z2, A2, X2 = C.pb2[0], C.pb2[1], C.pb2[2]
    kz, kA, kX = "pz", "pA", "pX"

    def do_group(g, it):
        npair = 2 * g + 2
        pO, kO = C.pb[6 + (g % 2)], ("pb", 6 + (g % 2))
        qs = qT[:, g * TB:(g + 1) * TB]

        def stage1(pi, i):
            j0 = 4 * g + 3 - 2 * pi
            Ei, Li = E[i % 2], L[i % 2]
            for u in range(2):
                jb = j0 - u
                P.op("pe", lambda e, u=u, jb=jb: e.matmul(pz[u][:], lhsT=kT[:, jb * 128:(jb + 1) * 128], rhs=qs, start=True, stop=True),
                     r=["qT", "kT"], w=[kz])
            P.op("act", lambda e: e.activation(out=Ei[:].rearrange("p u t -> p (u t)"), in_=z2[:], func=AF.Exp),
                 r=[kz], w=[("E", i % 2)])
            if pi < 2:
                P.op("dve", lambda e: e.tensor_tensor(out=Ei[:], in0=Ei[:], in1=mk[:, 2 * pi:2 * pi + 2, :], op=ALU.mult),
                     r=[("E", i % 2), "mk"], w=[("E", i % 2)])
            P.op("act", lambda e: e.activation(out=Li[:], in_=Ei[:], func=AF.Ln, bias=1.0, scale=1.0),
                 r=[("E", i % 2)], w=[("L", i % 2)])

        def stage2(pi, i):
            j0 = 4 * g + 3 - 2 * pi
            first, last = (pi == 0), (pi == npair - 1)
            Ei, Li, Wi = E[i % 2], L[i % 2], W[i % 2]
            lk = ("L", i % 2)

            def mm(bank, m, u, st):
                P.op("pe", lambda e: e.matmul(bank[:], lhsT=m[:], rhs=Li[:, u, :], start=st, stop=False, skip_group_check=True),
                     r=[lk, "tri", "tric", "onesm"], w=[kA])
            mm(pA[0], tri, 0, first)
            mm(pA[1], onesm, 0, first)
            mm(pA[1], tri, 1, False)
            P.op("act", lambda e: e.activation(out=X2[:], in_=A2[:], func=AF.Exp, scale=-1.0), r=[kA], w=[kX])
            if not last:
                mm(pA[0], tric, 0, False)
                mm(pA[0], onesm, 1, False)
                mm(pA[1], tric, 1, False)
            P.op("dve", lambda e: e.tensor_tensor(out=Wi[:].rearrange("p u t -> p (u t)"), in0=Ei[:].rearrange("p u t -> p (u t)"),
                                                  in1=X2[:], op=ALU.mult),
                 r=[("E", i % 2), kX], w=[("W", i % 2)])
            for u in range(2):
                jb = j0 - u
                P.op("pe", lambda e, u=u, jb=jb: e.matmul(pO[0:64, :], lhsT=V[:, jb, :], rhs=Wi[:, u, :],
                                                          start=(first and u == 0), stop=(last and u == 1)),
                     r=[("W", i % 2), "V"], w=[kO])

        stage1(0, it)
        for pi in range(npair):
            if pi + 1 < npair:
                stage1(pi + 1, it + pi + 1)
            stage2(pi, it + pi)
        o = ost[g % 2]
        P.op("act", lambda e, o=o, pO=pO: e.activation(out=o[:], in_=pO[0:64, :], func=AF.Copy), r=[kO], w=[("ost", g % 2)])
        P.op("sp", lambda e, o=o, g=g: e.dma_start(out=ysb_out.ap()[:, g * TB:(g + 1) * TB], in_=o[:]),
             r=[("ost", g % 2)], dma=True)
        return it + npair

    it = 0
    for g in range(NG):
        it = do_group(g, it)


def emit_ret(C, S, fmG, tmG, cst_d, yret_out):
    P = C.P
    C.reset()
    N = S // 128
    qT = C.sb("qT", [64, S], BF16)
    kT = C.sb("kT", [64, S], BF16)
    nl = S // 4 // 128
    V2 = C.sb("V", [128, N * 128], BF16)
    G2 = C.sb("G", [128, N * 128], BF16)
    V = V2[:, :].rearrange("p (n d) -> p n d", d=128)
    G = G2[:, :].rearrange("p (n d) -> p n d", d=128)
    stb = C.sb("stb", [64, N, 128], BF16)
    st = C.sb("st", [64, 128], F32)
    dec = C.sb("dec", [128, 128], F32)
    qdec = C.sb("qdec", [64, 128], F32)
    kdec = C.sb("kdec", [128, 1], F32)
    cdv = C.sb("cdv", [64, 1], F32)
    idn = C.sb("idn", [128, 128], BF16)
    load_fm(C, qT, fmG, S, 2, "qT")
    load_fm(C, kT, fmG, S, 3, "kT")
    load_tm(C, V2, tmG, S, 1, 0, nl * 128, "V")
    load_tm(C, G2, tmG, S, 2, 0, nl * 128, "G")
    for nm, t in (("dec", dec), ("qdec", qdec), ("kdec", kdec), ("cdv", cdv), ("idn", idn)):
        P.op("sp", lambda e, nm=nm, t=t: e.dma_start(out=t[:], in_=cst_d[nm].ap()), w=[nm], dma=True)
    P.op("dve", lambda e: e.memset(st[:], 0.0), w=["st"])
    Kd = [C.sb("Kd", [128, 64], BF16) for _ in range(2)]
    for n in range(N):
        i = n % 2
        P.op("dve", lambda e, n=n: e.tensor_copy(out=stb[:, n, :], in_=st[:]), r=["st"], w=[("stb", n)])
        if n == N - 1:
            break
        ptr, ktr = C.pbh[i], ("pb", i)
        P.op("pe", lambda e, n=n, ptr=ptr: e.transpose(out=ptr[:, 0:64], in_=kT[:, n * 128:(n + 1) * 128], identity=idn[0:64, 0:64]),
             r=["kT", "idn"], w=[ktr])
        P.op("dve", lambda e, i=i, ptr=ptr: e.tensor_scalar(out=Kd[i][:], in0=ptr[:, 0:64], scalar1=kdec[:, 0:1], scalar2=None,
                                                            op0=ALU.mult), r=[ktr, "kdec"], w=[("Kd", i)])
        pkv, kkv = C.pb[2 + i], ("pb", 2 + i)
        P.op("pe", lambda e, n=n, i=i, pkv=pkv: e.matmul(pkv[0:64, 0:128], lhsT=Kd[i][:], rhs=V[:, n, :], start=True, stop=True),
             r=[("Kd", i), "V"], w=[kkv])
        P.op("dve", lambda e, pkv=pkv: e.scalar_tensor_tensor(out=st[:], in0=st[:], scalar=cdv[:, 0:1], in1=pkv[0:64, 0:128],
                                                              op0=ALU.mult, op1=ALU.add), r=["st", kkv, "cdv"], w=["st"])
    Sm = [C.sb("Sm", [128, 128], BF16) for _ in range(2)]
    Qd = [C.sb("Qd", [64, 128], BF16) for _ in range(2)]
    ysq = [C.sb("ysq", [128, 128], F32) for _ in range(2)]
    ss = [C.sb("ss", [128, 1], F32) for _ in range(2)]
    lr = [C.sb("lr", [128, 1], F32) for _ in range(2)]
    rr = [C.sb("rr", [128, 1], F32) for _ in range(2)]
    yo = [C.sb("yo", [128, 128], BF16) for _ in range(2)]
    yst = [C.sb("yst", [128, TB], BF16) for _ in range(2)]
    for n in range(N):
        i = n % 2
        psc, ksc = C.pb[4 + i], ("pb", 4 + i)
        P.op("pe", lambda e, n=n, psc=psc: e.matmul(psc[:, 0:128], lhsT=kT[:, n * 128:(n + 1) * 128], rhs=qT[:, n * 128:(n + 1) * 128],
                                                    start=True, stop=True), r=["kT", "qT"], w=[ksc])
        P.op("dve", lambda e, i=i, psc=psc: e.tensor_tensor(out=Sm[i][:], in0=psc[:, 0:128], in1=dec[:], op=ALU.mult),
             r=[ksc, "dec"], w=[("Sm", i)])
        P.op("pool", lambda e, i=i, n=n: e.tensor_tensor(out=Qd[i][:], in0=qT[:, n * 128:(n + 1) * 128], in1=qdec[:], op=ALU.mult),
             r=["qT", "qdec"], w=[("Qd", i)])
        py, ky = C.pb[6 + i], ("pb", 6 + i)
        P.op("pe", lambda e, i=i, n=n, py=py: e.matmul(py[:, 0:128], lhsT=Sm[i][:], rhs=V[:, n, :], start=True, stop=False),
             r=[("Sm", i), "V"], w=[ky])
        P.op("pe", lambda e, i=i, n=n, py=py: e.matmul(py[:, 0:128], lhsT=Qd[i][:], rhs=stb[:, n, :], start=False, stop=True),
             r=[("Qd", i), ("stb", n)], w=[ky])
        P.op("act", lambda e, i=i, py=py: e.activation(out=ysq[i][:], in_=py[:, 0:128], func=AF.Square, accum_out=ss[i][:]),
             r=[ky], w=[("ysq", i), ("ss", i)])
        P.op("act", lambda e, i=i: e.activation(out=lr[i][:], in_=ss[i][:], func=AF.Ln, bias=EPS, scale=1.0 / 128),
             r=[("ss", i)], w=[("lr", i)])
        P.op("act", lambda e, i=i: e.activation(out=rr[i][:], in_=lr[i][:], func=AF.Exp, scale=-0.5),
             r=[("lr", i)], w=[("rr", i)])
        P.op("dve", lambda e, i=i, n=n, py=py: e.scalar_tensor_tensor(out=yo[i][:], in0=py[:, 0:128], scalar=rr[i][:, 0:1], in1=G[:, n, :],
                                                               op0=ALU.mult, op1=ALU.mult), r=[ky, ("rr", i), "G"], w=[("yo", i)])
        pt, kt = C.pbh[i], ("pb", i)
        P.op("pe", lambda e, i=i, pt=pt: e.transpose(out=pt[:, 0:128], in_=yo[i][:], identity=idn[:]), r=[("yo", i), "idn"], w=[kt])
        gi = (n // 4) % 2
        P.op("act", lambda e, n=n, gi=gi, pt=pt: e.activation(out=yst[gi][:, (n % 4) * 128:(n % 4 + 1) * 128], in_=pt[:, 0:128], func=AF.Copy),
             r=[kt], w=[("yst", gi)])
        if n % 4 == 3:
            P.op("sp", lambda e, n=n, gi=gi: e.dma_start(out=yret_out.ap()[:, (n - 3) * 128:(n + 1) * 128], in_=yst[gi][:]),
                 r=[("yst", gi)], dma=True)


def emit_pool(C, S, tmG, wp_d, ps_d, cst_d, ypool_out):
    P = C.P
    C.reset()
    N = S // 128
    nl = S // 4 // 128
    U2 = C.sb("U", [128, N * 64], BF16)
    U = U2[:, :].rearrange("p (n d) -> p n d", d=64)
    wp = C.sb("wp", [64, 64], BF16)
    psc = C.sb("psc", [64, 1], F32)
    b0 = C.sb("b0", [128, 128], BF16)
    b0f = C.sb("b0f", [128, 128], BF16)
    b1 = C.sb("b1", [128, 128], BF16)
    load_tm(C, U2, tmG, S, 0, nl * 64, nl * 64, "U")
    P.op("pool", lambda e: e.dma_start(out=wp[:], in_=wp_d.ap()), w=["wp"], dma=True)
    P.op("sp", lambda e: e.dma_start(out=psc[:], in_=ps_d.ap()), w=["psc"], dma=True)
    for nm, t in (("b0", b0), ("b0f", b0f), ("b1", b1)):
        P.op("sp", lambda e, nm=nm, t=t: e.dma_start(out=t[:], in_=cst_d[nm].ap()), w=[nm], dma=True)
    zb = [C.sb("zb", [64, TB], BF16) for _ in range(2)]
    yp = [C.sb("yp", [64, TB], BF16) for _ in range(2)]
    for g in range(S // TB):
        i = g % 2
        pz, kz = C.pb[i], ("pb", i)
        for s in range(4):
            n = 4 * g + s
            cur = b0f if n == 0 else b0
            P.op("pe", lambda e, n=n, s=s, cur=cur, pz=pz: e.matmul(pz[0:64, s * 128:(s + 1) * 128], lhsT=U[:, n, :], rhs=cur[:],
                                                                    start=True, stop=(n == 0)), r=["U", "b0", "b0f"], w=[kz])
            if n > 0:
                P.op("pe", lambda e, n=n, s=s, pz=pz: e.matmul(pz[0:64, s * 128:(s + 1) * 128], lhsT=U[:, n - 1, :], rhs=b1[:],
                                                               start=False, stop=True), r=["U", "b1"], w=[kz])
        P.op("dve", lambda e, i=i, pz=pz: e.tensor_copy(out=zb[i][:], in_=pz[0:64, :]), r=[kz], w=[("zb", i)])
        py, ky = C.pb[2 + i], ("pb", 2 + i)
        P.op("pe", lambda e, i=i, py=py: e.matmul(py[0:64, :], lhsT=wp[:], rhs=zb[i][:], start=True, stop=True),
             r=[("zb", i), "wp"], w=[ky])
        P.op("dve", lambda e, i=i, py=py: e.tensor_scalar(out=yp[i][:], in0=py[0:64, :], scalar1=psc[:, 0:1], scalar2=None, op0=ALU.mult),
             r=[ky, "psc"], w=[("yp", i)])
        P.op("sp", lambda e, i=i, g=g: e.dma_start(out=ypool_out.ap()[:, g * TB:(g + 1) * TB], in_=yp[i][:]), r=[("yp", i)], dma=True)


def rope_tables(S, T, j):
    half = 32
    pos = np.arange(j * T, (j + 1) * T, dtype=np.float32)
    inv_freq = (np.float32(10000.0) ** (-np.arange(half, dtype=np.float32) / np.float32(half))).astype(np.float32)
    ang = (pos[:, None] * inv_freq[None, :]).astype(np.float32)
    c = np.cos(ang).astype(np.float32).T
    s = np.sin(ang).astype(np.float32).T
    cos64 = np.concatenate([c, c], 0)
    sin64 = np.concatenate([-s, s], 0)
    cos = np.concatenate([cos64, cos64 * 0.125], 0)
    sin = np.concatenate([sin64, sin64 * 0.125], 0)
    return np.ascontiguousarray(cos, np.float32), np.ascontiguousarray(sin, np.float32)


def mixer_consts(h):
    j = np.arange(128)
    tri = (j[:, None] >= j[None, :]).astype(np.float32)
    tric = 1.0 - tri
    c = np.arange(TB)
    mask = np.stack([(c[None, :] > (128 * r + j[:, None])).astype(np.float32) for r in (3, 2, 1, 0)], 1)
    lg = np.log(np.float32(1.0) - np.float32(2.0) ** np.float32(-5.0 - h)).astype(np.float32)
    idx = np.arange(128, dtype=np.float32)
    diff = idx[None, :] - idx[:, None]
    dec = np.where(diff >= 0, np.exp(np.where(diff >= 0, diff, 0.0) * lg), 0.0).astype(np.float32)
    qdec = np.tile(np.exp((idx + 1) * lg)[None, :], (64, 1)).astype(np.float32)
    kdec = np.exp((127 - idx) * lg)[:, None].astype(np.float32)
    cdv = np.full((64, 1), np.exp(128 * lg), np.float32)
    w = POOL_WINDOWS[h]
    t = np.arange(128)
    s_ = np.arange(128)
    inwin = ((s_[:, None] <= t[None, :]) & (s_[:, None] > t[None, :] - w)).astype(np.float32)
    eye = np.eye(128, dtype=np.float32)
    b0 = inwin / w - eye
    cnt = np.minimum(t + 1, w).astype(np.float32)
    b0f = inwin / cnt[None, :] - eye
    b1 = (((s_[:, None] - 128) > (t[None, :] - w))).astype(np.float32) / w
    return dict(tri=tri.astype(NPBF), tric=tric.astype(NPBF), mask=mask.reshape(128, 4 * TB).astype(NPBF),
                dec=dec, qdec=qdec, kdec=kdec, cdv=cdv, idn=np.eye(128, dtype=np.float32).astype(NPBF),
                b0=b0.astype(NPBF), b0f=b0f.astype(NPBF), b1=b1.astype(NPBF))


CONST_SPECS = dict(tri=([128, 128], BF16), tric=([128, 128], BF16), mask=([128, 4 * TB], BF16), dec=([128, 128], F32),
                   qdec=([64, 128], F32), kdec=([128, 1], F32), cdv=([64, 1], F32), idn=([128, 128], BF16),
                   b0=([128, 128], BF16), b0f=([128, 128], BF16), b1=([128, 128], BF16))


def gcol_layout(g):
    return np.ascontiguousarray(g.reshape(NK, 128).T, np.float32)


def perm_w_in(w):
    o = {"q_sb": 0, "k_sb": 256, "v_sb": 512, "q_r": 768, "k_r": 1024, "v_r": 1280, "g_r": 1792, "u_p": 2304, "gate": 2560}
    cols = []
    for h in range(4):
        cols += list(range(o["q_sb"] + 64 * h, o["q_sb"] + 64 * h + 64))
        cols += list(range(o["k_sb"] + 64 * h, o["k_sb"] + 64 * h + 64))
        cols += list(range(o["q_r"] + 64 * h, o["q_r"] + 64 * h + 64))
        cols += list(range(o["k_r"] + 64 * h, o["k_r"] + 64 * h + 64))
        for base in (o["q_r"], o["k_r"]):
            cols += list(range(base + 64 * h + 32, base + 64 * h + 64))
            cols += list(range(base + 64 * h, base + 64 * h + 32))
    cols += list(range(o["v_sb"], o["v_sb"] + 256))
    cols += list(range(o["u_p"], o["u_p"] + 256))
    cols += list(range(o["v_r"], o["v_r"] + 512))
    cols += list(range(o["g_r"], o["g_r"] + 512))
    assert len(cols) == 3072
    return np.ascontiguousarray(w[:, cols]), np.ascontiguousarray(w[:, o["gate"]:])


GROUPS = [[0, 1, 2, 3], [4, 5, 6, 7]]
CC_MAX_ELEMS = 524288


def y_chunk_rows(rows, S):
    return min(rows, max(1, CC_MAX_ELEMS // S))


def allgather(C, src, dst, r0, nrows, rkeys, wkeys):
    C.P.op("pool", lambda e: e.collective_compute("AllGather", ALU.bypass, replica_groups=GROUPS,
                                                 ins=[src.ap()[r0:r0 + nrows, :].opt()],
                                                 outs=[dst.ap()[4 * r0:4 * (r0 + nrows), :].opt()]),
           r=rkeys, w=wkeys, dma=True, cc=True)


def perm_branch_rows(w, S):
    rows = w.shape[0] // 4
    pr = y_chunk_rows(rows, S)
    idx = [r * rows + c * pr + p for c in range(rows // pr) for r in range(4) for p in range(pr)]
    return w[idx]


def gather_y(C, yb, yg, S):
    rows = yb.shape[0]
    pr = y_chunk_rows(rows, S)
    for c in range(rows // pr):
        allgather(C, yb, yg, c * pr, pr, [], [])


def build_program(S, depth, stop=99):
    T = S // 4
    nl = T // 128
    C = Ctx()
    x_in = C.din("x_in", [D, T], F32)
    x_out = C.dout("x_out", [D, T], F32)
    xs = C.dint("xs", [D, T], F32)
    cos = C.din("p_cos", [128, T], F32)
    sin = C.din("p_sin", [128, T], F32)
    scv = C.din("p_scv", [128, 1], F32)
    gfin = C.din("gfin", [128, NK], F32)
    cst = {k: C.din("c_" + k, shp, dt) for k, (shp, dt) in CONST_SPECS.items()}
    fmA = C.dint("fmA", [D, T], BF16)
    fmG = C.dint("fmG", [4 * D, T], BF16)
    tmA = C.dint("tmA", [1536, nl * 128], BF16)
    tmG = C.dint("tmG", [4 * 1536, nl * 128], BF16)
    nb = T // TB
    fmkeys = [("fmA", b) for b in range(nb)]
    yB = [C.dint("ysbB", [64, S], BF16), C.dint("yretB", [128, S], BF16), C.dint("ypoolB", [64, S], BF16)]
    yG = [C.dint("ysbG", [256, S], BF16), C.dint("yretG", [512, S], BF16), C.dint("ypoolG", [256, S], BF16)]
    yL = [C.dint("ysbL", [256, T], BF16), C.dint("yretL", [512, T], BF16), C.dint("ypoolL", [256, T], BF16)]
    cur = x_in
    for l in range(depth):
        C.dq = "sp" if l % 2 == 0 else "act"
        W = {n: C.din(f"{n}{l}", shp, F32) for n, shp in (
            ("f1g", [128, NK]), ("f1w1", [D, FF]), ("f1w3", [D, FF]), ("f1w2", [FF, D]),
            ("pg", [128, NK]), ("pwq", [D, 3072]), ("mwg", [D, 3072]), ("mwb", [D, D]), ("mwo", [D, D]),
            ("f2g", [128, NK]), ("f2w1", [D, FF]), ("f2w3", [D, FF]), ("f2w2", [FF, D]),
            ("wp", [64, 64]), ("psc", [64, 1]))}
        emit_ffn(C, T, cur, xs, W["f1g"], W["f1w1"], W["f1w3"], W["f1w2"], None)
        if stop <= 1: break
        emit_proj(C, T, xs, W["pg"], W["pwq"], cos, sin, scv, fmA, tmA)
        C.P.barrier()
        if stop <= 2: break
        for h in range(4):
            allgather(C, fmA, fmG, 2 * h * 128, 128, fmkeys, [("fmG", 2 * h)])
        for h in range(4):
            allgather(C, tmA, tmG, (h * 3) * 128, 128, ["tmA"], [("tmG", 0, h)])
        C.P.barrier()
        for h in range(4):
            allgather(C, fmA, fmG, (2 * h + 1) * 128, 128, fmkeys, [("fmG", 2 * h + 1)])
        for g in (1, 2):
            for h in range(4):
                allgather(C, tmA, tmG, (h * 3 + g) * 128, 128, ["tmA"], [("tmG", g, h)])
        if stop <= 3: break
        C.soft_reset = True
        emit_sb(C, S, fmG, tmG, cst, yB[0])
        if stop <= 4: break
        C.P.barrier()
        gather_y(C, yB[0], yG[0], S)
        C.skip_cc = True
        C.soft_reset = False
        emit_ret(C, S, fmG, tmG, cst, yB[1])
        if stop <= 5: break
        C.P.barrier(skip_cc=True)
        gather_y(C, yB[1], yG[1], S)
        emit_pool(C, S, tmG, W["wp"], W["psc"], cst, yB[2])
        C.skip_cc = False
        C.P.barrier()
        if stop <= 6: break
        gather_y(C, yB[2], yG[2], S)
        if stop <= 7: break
        emit_merge(C, T, xs, xs, W["pg"], W["mwg"], W["mwb"], W["mwo"], yG, yL)
        last = (l == depth - 1)
        emit_ffn(C, T, xs, x_out if last else xs, W["f2g"], W["f2w1"], W["f2w3"], W["f2w2"], gfin if last else None)
        cur = xs
    C.P.barrier()
    C.P.emit()
    return C


def forward(x, g_ffn1, w1_ffn1, w3_ffn1, w2_ffn1, g_mix, w_in, w_branch_sb, w_branch_ret, w_branch_pool, w_pool,
            pool_scale, w_out, g_ffn2, w1_ffn2, w3_ffn2, w2_ffn2, g_final):
    B, S, _ = x.shape
    depth = w_in.shape[0]
    assert B == 2
    T = S // 4
    f32 = lambda a: np.ascontiguousarray(np.asarray(a, np.float32))
    xf = f32(x).reshape(B * S, D)
    common = dict(p_scv=np.concatenate([np.full((64, 1), 0.125, np.float32), np.ones((64, 1), np.float32)], 0),
                  gfin=gcol_layout(f32(g_final)))
    for l in range(depth):
        wq, wg = perm_w_in(f32(w_in[l]))
        common.update({
            f"f1g{l}": gcol_layout(f32(g_ffn1[l])), f"f1w1{l}": f32(w1_ffn1[l]), f"f1w3{l}": f32(w3_ffn1[l]),
            f"f1w2{l}": f32(w2_ffn1[l]), f"pg{l}": gcol_layout(f32(g_mix[l])), f"pwq{l}": wq, f"mwg{l}": wg,
            f"mwb{l}": f32(np.concatenate([perm_branch_rows(f32(w_branch_sb[l]), S), perm_branch_rows(f32(w_branch_ret[l]), S),
                                           perm_branch_rows(f32(w_branch_pool[l]), S)], 0)),
            f"mwo{l}": f32(w_out[l]), f"f2g{l}": gcol_layout(f32(g_ffn2[l])), f"f2w1{l}": f32(w1_ffn2[l]),
            f"f2w3{l}": f32(w3_ffn2[l]), f"f2w2{l}": f32(w2_ffn2[l])})
    in_maps = []
    for c in range(NCORES):
        h = c % 4
        m = dict(common)
        m["x_in"] = np.ascontiguousarray(xf[c * T:(c + 1) * T].T)
        m["p_cos"], m["p_sin"] = rope_tables(S, T, c % 4)
        m.update({"c_" + k: v for k, v in mixer_consts(h).items()})
        for l in range(depth):
            m[f"wp{l}"] = f32(w_pool[l][h])
            m[f"psc{l}"] = f32(np.asarray(pool_scale[l], np.float32)[h * 64:(h + 1) * 64, None])
        in_maps.append(m)
    import os
    C = build_program(S, depth, int(os.environ.get("KSTOP", "99")))
    if os.environ.get("KTRACE"):
        rr = run_bass_kernel_spmd(C.nc, in_maps, core_ids=list(range(NCORES)), trace=True)
        print("KTRACE exec_time_ns", rr.exec_time_ns)
        res = rr.results
    else:
        res = run_bass_kernel_spmd(C.nc, in_maps, core_ids=list(range(NCORES))).results
    outf = np.concatenate([res[c]["x_out"].T for c in range(NCORES)], 0)
    return np.ascontiguousarray(outf.reshape(B, S, D).astype(np.float32))


def kernel(**inputs):
    inputs = {k: np.asarray(v) for k, v in inputs.items()}
    return forward(**inputs)
```

```python
import numpy as np
import ml_dtypes
import concourse.bass as bass
import concourse.mybir as mybir
from concourse.bass_utils import run_bass_kernel_spmd

F32 = mybir.dt.float32
BF16 = mybir.dt.bfloat16
AF = mybir.ActivationFunctionType
ALU = mybir.AluOpType
NPBF = ml_dtypes.bfloat16

D = 1024
FF = 2816
NK = 8
NF = 22
EPS = 1e-6
TB = 512
NCORES = 8
DEPTH = 2
BATCH = 2
POOL_WINDOWS = (2, 4, 8, 16)

SEM_EPOCH = 30000
N_DMA_SEMS = 6


class Prog:
    ENGS = ("pe", "act", "dve", "pool", "sp")

    def __init__(self, nc):
        self.nc = nc
        self.ops = []
        self.last_w = {}
        self.readers = {}
        self.n_sig = {e: 0 for e in self.ENGS}
        self.n_dma = {e: 0 for e in self.ENGS}
        self.n_cc = 0
        self.last_cc = None

    def op(self, eng, fn, r=(), w=(), dma=False, extra_deps=(), cc=False):
        deps = set(extra_deps)
        for k in r:
            if k in self.last_w:
                deps.add(self.last_w[k])
        for k in w:
            if k in self.last_w:
                deps.add(self.last_w[k])
            for v in self.readers.get(k, {}).values():
                deps.add(v)
        idx = len(self.ops)
        o = dict(eng=eng, fn=fn, deps=deps, dma=dma, idx=idx, cc=cc)
        if cc:
            self.n_cc += 1
            o["ccval"] = self.n_cc
            if self.last_cc is not None:
                deps.add(self.last_cc)
            self.last_cc = idx
        elif dma:
            n = self.n_dma[eng]
            self.n_dma[eng] = n + 1
            o["dslot"] = n % N_DMA_SEMS
            o["dval"] = 16 * (n // N_DMA_SEMS + 1)
            if n >= N_DMA_SEMS:
                o["prev_same_slot"] = True
        else:
            self.n_sig[eng] += 1
            o["sig"] = self.n_sig[eng]
        self.ops.append(o)
        for k in r:
            rk = ("dma", idx) if dma else eng
            self.readers.setdefault(k, {})[rk] = idx
        for k in w:
            self.last_w[k] = idx
            self.readers[k] = {}
        return idx

    def hid(self, e):
        k = id(e)
        if k not in self.hid_cache:
            self.hid_cache[k] = e.partition_id() % 4
        return self.hid_cache[k]

    def barrier(self, skip_cc=False):
        last = {}
        for o in self.ops:
            if o["cc"]:
                if not skip_cc:
                    last[("cc",)] = o["idx"]
            elif o["dma"]:
                last[("dma", o["eng"], o["dslot"])] = o["idx"]
            else:
                last[o["eng"]] = o["idx"]
        deps = set(last.values())
        for e in self.ENGS:
            self.op(e, None, extra_deps=deps)
        self.last_w = {}
        self.readers = {}

    def emit(self):
        nc = self.nc
        n_epochs = {e: (self.n_sig[e] // SEM_EPOCH) + 1 for e in self.ENGS}
        csem = {e: [nc.alloc_semaphore(f"c_{e}_{i}") for i in range(n_epochs[e])] for e in self.ENGS}
        dsem = {e: [nc.alloc_semaphore(f"d_{e}_{i}") for i in range(N_DMA_SEMS)]
                for e in self.ENGS if self.n_dma[e] > 0}
        ccsem = nc.alloc_semaphore("cc_sem") if self.n_cc else None

        def sig_of(o):
            if o["cc"]:
                return (("cc",), ccsem, o["ccval"])
            if o["dma"]:
                return (("d", o["eng"], o["dslot"]), dsem[o["eng"]][o["dslot"]], o["dval"])
            s = o["sig"]
            ep = (s - 1) // SEM_EPOCH
            return (("c", o["eng"], ep), csem[o["eng"]][ep], s - ep * SEM_EPOCH)

        per_eng = {e: [] for e in self.ENGS}
        for o in self.ops:
            per_eng[o["eng"]].append(o)
        ops = self.ops

        def build(ename):
            def body(e):
                self.hid_cache = {}
                waited = {}
                for o in per_eng[ename]:
                    waits = {}
                    for d in o["deps"]:
                        do = ops[d]
                        same = (not do["dma"]) and do["eng"] == ename
                        if same and ename == "pe" and not o["dma"]:
                            continue
                        if same and do["fn"] is None:
                            continue
                        key, sem, val = sig_of(do)
                        if waits.get(key, (None, 0))[1] < val:
                            waits[key] = (sem, val)
                    if o.get("prev_same_slot"):
                        key = ("d", ename, o["dslot"])
                        val = o["dval"] - 16
                        if waits.get(key, (None, 0))[1] < val:
                            waits[key] = (dsem[ename][o["dslot"]], val)
                    for key, (sem, val) in waits.items():
                        if waited.get(key, 0) >= val:
                            continue
                        waited[key] = val
                        e.wait_ge(sem, val)
                    ins = e.nop() if o["fn"] is None else o["fn"](e)
                    key, sem, val = sig_of(o)
                    if o["cc"]:
                        ins.then_inc(sem)
                    elif o["dma"]:
                        ins.then_inc(sem, 16)
                    else:
                        ins.then_inc(sem, 1)
            return body

        with nc.Block() as block:
            block.sync(build("sp"))
            block.tensor(build("pe"))
            block.scalar(build("act"))
            block.vector(build("dve"))
            block.gpsimd(build("pool"))


class Ctx:
    def __init__(self):
        self.nc = bass.Bass("TRN2", target_bir_lowering=False)
        self.P = Prog(self.nc)
        self.base = 16512
        self.top = 229344
        self.off = self.base
        self.uid = 0
        self.pb2 = [self.nc.alloc_psum_tensor(f"pb2_{i}", [128, 1024], F32) for i in range(4)]
        self.pb = [self.pb2[i // 2][:, (i % 2) * 512:(i % 2 + 1) * 512] for i in range(8)]
        self.pbh = [self.pb2[i // 2].bitcast(BF16)[:, (i % 2) * 1024:(i % 2) * 1024 + 1024] for i in range(8)]
        self.soft_reset = False
        self.skip_cc = False

    def reset(self):
        if self.soft_reset:
            self.soft_reset = False
        else:
            self.P.barrier(skip_cc=self.skip_cc)
        self.off = self.base

    def sb(self, name, shape, dtype):
        n = 1
        for s in shape[1:]:
            n *= s
        nbytes = n * (4 if dtype == F32 else 2)
        off = (self.off + 31) // 32 * 32
        assert off + nbytes <= self.top, (name, off, nbytes, self.top)
        self.off = off + nbytes
        self.uid += 1
        return self.nc.alloc_sbuf_tensor_at(f"{name}_{self.uid}", list(shape), dtype, offset=off)

    def din(self, name, shape, dtype):
        return self.nc.dram_tensor(name, list(shape), dtype, kind="ExternalInput")

    def dout(self, name, shape, dtype):
        return self.nc.dram_tensor(name, list(shape), dtype, kind="ExternalOutput")

    def dint(self, name, shape, dtype):
        return self.nc.dram_tensor(name, list(shape), dtype)


def xblk(x_d, b):
    return x_d.ap().rearrange("(k p) t -> p k t", p=128)[:, :, b * TB:(b + 1) * TB]


def emit_consts(C):
    P = C.P
    ones = C.sb("ones", [128, 128], BF16)
    P.op("pool", lambda e: e.memset(ones[:], 1.0), w=["ones"])
    return ones


def emit_rms(C, xb, xkey, ones, gcol, out_fn, sq, rs, stat_bank, tag):
    P = C.P
    pst = C.pb[stat_bank]
    for k in range(NK):
        s = sq[k % 2]
        sk = ("sq", k % 2)
        P.op("pool", lambda e, s=s, k=k: e.tensor_tensor(out=s[:], in0=xb[:, k, :], in1=xb[:, k, :], op=ALU.mult),
             r=[xkey], w=[sk])
        P.op("pe", lambda e, s=s, k=k: e.matmul(pst[:], lhsT=ones[:], rhs=s[:], start=(k == 0), stop=(k == NK - 1)),
             r=[sk, "ones"], w=[("pb", stat_bank)])
    lnv, rstd = rs
    P.op("act", lambda e: e.activation(out=lnv[:], in_=pst[:], func=AF.Ln, bias=EPS, scale=1.0 / D),
         r=[("pb", stat_bank)], w=["lnv"])
    P.op("act", lambda e: e.activation(out=rstd[:], in_=lnv[:], func=AF.Exp, scale=-0.5),
         r=["lnv"], w=["rstd"])
    for k in range(NK):
        oap, okeys = out_fn(k)
        P.op("dve", lambda e, k=k, oap=oap: e.scalar_tensor_tensor(out=oap, in0=xb[:, k, :], scalar=gcol[:, k:k + 1],
                                                                   in1=rstd[:], op0=ALU.mult, op1=ALU.mult),
             r=[xkey, "rstd", "gcol" + tag], w=okeys)


def load_gcol(C, g_d, tag):
    gcol = C.sb("gcol" + tag, [128, NK], F32)
    C.P.op("sp", lambda e: e.dma_start(out=gcol[:], in_=g_d.ap()), w=["gcol" + tag], dma=True)
    return gcol


def load_w_cast(C, dst_ap, src_ap, key):
    C.P.op("pool", lambda e: e.dma_start(out=dst_ap, in_=src_ap, max_dma_last_dim=4096), w=[key], dma=True)


def emit_ffn(C, T, x_src, x_dst, g_d, w1_d, w3_d, w2_d, gfin_d=None):
    P = C.P
    C.reset()
    nb = T // TB
    ones = emit_consts(C)
    gcol = load_gcol(C, g_d, "f")
    gfin = load_gcol(C, gfin_d, "fin") if gfin_d is not None else None
    w1s = C.sb("w1s", [128, NK, FF], BF16)
    w3s = C.sb("w3s", [128, NK, FF], BF16)
    w2s = C.sb("w2s", [128, NF, D], BF16)
    for k in range(NK):
        load_w_cast(C, w1s[:, k, :], w1_d.ap()[k * 128:(k + 1) * 128, :], ("w1", k))
        load_w_cast(C, w3s[:, k, :], w3_d.ap()[k * 128:(k + 1) * 128, :], ("w3", k))
    for f in range(NF):
        load_w_cast(C, w2s[:, f, :], w2_d.ap()[f * 128:(f + 1) * 128, :], ("w2", f))
    xbs = [C.sb("xb", [128, NK, TB], F32) for _ in range(2)]
    hT = C.sb("hT", [128, NK, TB], BF16)
    gT = C.sb("gT", [128, NF, TB], BF16)
    sq = [C.sb("sq", [128, TB], BF16) for _ in range(2)]
    rs = (C.sb("lnv", [128, TB], F32), C.sb("rstd", [128, TB], F32))
    sil = [C.sb("sil", [128, TB], F32) for _ in range(2)]
    w1keys = [("w1", k) for k in range(NK)]
    w3keys = [("w3", k) for k in range(NK)]

    def load_x(b):
        xb = xbs[b % 2]
        P.op("sp", lambda e, xb=xb, b=b: e.dma_start(out=xb[:], in_=xblk(x_src, b)), r=[("xd", b)], w=[("xb", b % 2)], dma=True)

    def do_block(b):
        xb = xbs[b % 2]
        xkey = ("xb", b % 2)
        if b + 1 < nb:
            load_x(b + 1)
        emit_rms(C, xb, xkey, ones, gcol, lambda k: (hT[:, k, :], [("hT", k)]), sq, rs, 0, "f")
        for f in range(NF):
            pa, pbb = C.pb[1 + (f % 2)], C.pb[3 + (f % 2)]
            ka, kb = ("pb", 1 + (f % 2)), ("pb", 3 + (f % 2))
            for k in range(NK):
                P.op("pe", lambda e, k=k, f=f, pa=pa: e.matmul(pa[:], lhsT=w1s[:, k, f * 128:(f + 1) * 128], rhs=hT[:, k, :],
                                                               start=(k == 0), stop=(k == NK - 1)),
                     r=[("hT", k), ("w1", k)], w=[ka])
            for k in range(NK):
                P.op("pe", lambda e, k=k, f=f, pbb=pbb: e.matmul(pbb[:], lhsT=w3s[:, k, f * 128:(f + 1) * 128], rhs=hT[:, k, :],
                                                                 start=(k == 0), stop=(k == NK - 1)),
                     r=[("hT", k), ("w3", k)], w=[kb])
            s = sil[f % 2]
            P.op("act", lambda e, s=s, pa=pa: e.activation(out=s[:], in_=pa[:], func=AF.Silu), r=[ka], w=[("sil", f % 2)])
            P.op("dve", lambda e, s=s, pbb=pbb, f=f: e.tensor_tensor(out=gT[:, f, :], in0=s[:], in1=pbb[:], op=ALU.mult),
                 r=[("sil", f % 2), kb], w=[("gT", f)])
        for c in range(NK):
            py, ky = C.pb[5 + (c % 2)], ("pb", 5 + (c % 2))
            for f in range(NF):
                P.op("pe", lambda e, c=c, f=f, py=py: e.matmul(py[:], lhsT=w2s[:, f, c * 128:(c + 1) * 128], rhs=gT[:, f, :],
                                                               start=(f == 0), stop=(f == NF - 1)),
                     r=[("gT", f), ("w2", f)], w=[ky])
            P.op("dve", lambda e, c=c, py=py: e.scalar_tensor_tensor(out=xb[:, c, :], in0=py[:], scalar=0.5, in1=xb[:, c, :],
                                                                      op0=ALU.mult, op1=ALU.add),
                 r=[ky, xkey], w=[xkey])
        if gfin is not None:
            emit_rms(C, xb, xkey, ones, gfin, lambda k: (xb[:, k, :], [xkey]), sq, rs, 0, "fin")
        P.op("sp", lambda e, b=b, xb=xb: e.dma_start(out=xblk(x_dst, b), in_=xb[:]), r=[xkey], w=[("xd", b)], dma=True)

    load_x(0)
    for b in range(nb):
        do_block(b)


def emit_proj(C, T, x_src, g_d, wq_d, cos_d, sin_d, scv_d, fm_out, tm_out):
    P = C.P
    C.reset()
    nb = T // TB
    ones = emit_consts(C)
    gcol = load_gcol(C, g_d, "p")
    scv = C.sb("scv", [128, 1], F32)
    P.op("sp", lambda e: e.dma_start(out=scv[:], in_=scv_d.ap()), w=["scv"], dma=True)
    wq = C.sb("wq", [128, NK, 3072], BF16)
    for k in range(NK):
        load_w_cast(C, wq[:, k, :], wq_d.ap()[k * 128:(k + 1) * 128, :], ("wq", k))
    xbs = [C.sb("xb", [128, NK, TB], F32) for _ in range(2)]
    hT = C.sb("hT", [128, NK, TB], BF16)
    sq = [C.sb("sq", [128, TB], BF16) for _ in range(2)]
    rs = (C.sb("lnv", [128, TB], F32), C.sb("rstd", [128, TB], F32))
    cs = C.sb("cos", [128, TB], F32)
    sn = C.sb("sin", [128, TB], F32)
    t1 = C.sb("t1", [128, TB], F32)
    t2 = C.sb("t2", [128, TB], F32)
    fms = C.sb("fms", [128, 8, TB], BF16)
    nl = T // 128
    tms = C.sb("tms", [128, 4, 3, nl * 128], BF16)
    hkeys = [("hT", k) for k in range(NK)]
    wkeys = [("wq", k) for k in range(NK)]

    def load_x(b):
        xb = xbs[b % 2]
        P.op("sp", lambda e, xb=xb, b=b: e.dma_start(out=xb[:], in_=xblk(x_src, b)), r=[("xd", b)], w=[("xb", b % 2)], dma=True)

    def do_block(b):
        xb = xbs[b % 2]
        xkey = ("xb", b % 2)
        if b + 1 < nb:
            load_x(b + 1)
        P.op("sp", lambda e, b=b: e.dma_start(out=cs[:], in_=cos_d.ap()[:, b * TB:(b + 1) * TB]), w=["cos"], dma=True)
        P.op("sp", lambda e, b=b: e.dma_start(out=sn[:], in_=sin_d.ap()[:, b * TB:(b + 1) * TB]), w=["sin"], dma=True)
        emit_rms(C, xb, xkey, ones, gcol, lambda k: (hT[:, k, :], [("hT", k)]), sq, rs, 0, "p")

        def fm_mm(j, bank):
            for k in range(NK):
                P.op("pe", lambda e, k=k: e.matmul(C.pb[bank][:], lhsT=wq[:, k, j * 128:(j + 1) * 128], rhs=hT[:, k, :],
                                                   start=(k == 0), stop=(k == NK - 1)),
                     r=[("hT", k), ("wq", k)], w=[("pb", bank)])

        for h in range(4):
            fm_mm(3 * h, 1)
            P.op("dve", lambda e, h=h: e.tensor_scalar(out=fms[:, 2 * h, :], in0=C.pb[1][:], scalar1=scv[:, 0:1], scalar2=None,
                                                       op0=ALU.mult),
                 r=[("pb", 1), "scv"], w=[("fms", 2 * h)])
            fm_mm(3 * h + 1, 2)
            fm_mm(3 * h + 2, 3)
            P.op("dve", lambda e: e.tensor_tensor(out=t1[:], in0=C.pb[2][:], in1=cs[:], op=ALU.mult),
                 r=[("pb", 2), "cos"], w=["t1"])
            P.op("dve", lambda e: e.tensor_tensor(out=t2[:], in0=C.pb[3][:], in1=sn[:], op=ALU.mult),
                 r=[("pb", 3), "sin"], w=["t2"])
            P.op("pool", lambda e, h=h: e.tensor_tensor(out=fms[:, 2 * h + 1, :], in0=t1[:], in1=t2[:], op=ALU.add),
                 r=["t1", "t2"], w=[("fms", 2 * h + 1)])
        P.op("sp", lambda e, b=b: e.dma_start(out=fm_out.ap().rearrange("(j p) t -> p j t", p=128)[:, :, b * TB:(b + 1) * TB], in_=fms[:]),
             r=[("fms", j) for j in range(8)], w=[("fmA", b)], dma=True)
        for s in range(4):
            for j in range(3):
                bank = 4 + j
                for k in range(NK):
                    P.op("pe", lambda e, k=k, s=s, j=j, bank=bank: e.matmul(
                        C.pb[bank][:], lhsT=hT[:, k, s * 128:(s + 1) * 128], rhs=wq[:, k, 1536 + j * 512:1536 + (j + 1) * 512],
                        start=(k == 0), stop=(k == NK - 1)), r=[("hT", k), ("wq", k)], w=[("pb", bank)])
            n_l = 4 * b + s
            P.op("dve", lambda e, n_l=n_l: e.tensor_copy(out=tms[:, :, 0, n_l * 64:(n_l + 1) * 64],
                                                         in_=C.pb[4][:, 0:256].rearrange("p (h d) -> p h d", h=4)),
                 r=[("pb", 4)], w=[("tms", 0)])
            P.op("dve", lambda e, n_l=n_l: e.tensor_copy(out=tms[:, :, 0, nl * 64 + n_l * 64:nl * 64 + (n_l + 1) * 64],
                                                         in_=C.pb[4][:, 256:512].rearrange("p (h d) -> p h d", h=4)),
                 r=[("pb", 4)], w=[("tms", 1)])
            P.op("act", lambda e, n_l=n_l: e.activation(out=tms[:, :, 1, n_l * 128:(n_l + 1) * 128],
                                                        in_=C.pb[5][:].rearrange("p (h d) -> p h d", h=4), func=AF.Copy),
                 r=[("pb", 5)], w=[("tms", 2)])
            P.op("act", lambda e, n_l=n_l: e.activation(out=tms[:, :, 2, n_l * 128:(n_l + 1) * 128],
                                                        in_=C.pb[6][:].rearrange("p (h d) -> p h d", h=4), func=AF.Silu),
                 r=[("pb", 6)], w=[("tms", 3)])

    load_x(0)
    for b in range(nb):
        do_block(b)
    P.op("sp", lambda e: e.dma_start(out=tm_out.ap().rearrange("(h g p) x -> p h g x", h=4, g=3), in_=tms[:]),
         r=[("tms", i) for i in range(4)], w=["tmA"], dma=True)


def emit_merge(C, T, x_src, x_dst, g_d, wg_d, wb_d, wo_d, yG, yL):
    P = C.P
    C.reset()
    nb = T // TB
    ones = emit_consts(C)
    gcol = load_gcol(C, g_d, "m")
    wg = C.sb("wg", [128, NK, 3072], BF16)
    wb = C.sb("wb", [128, NK, D], BF16)
    wo = C.sb("wo", [128, NK, D], BF16)
    for k in range(NK):
        load_w_cast(C, wg[:, k, :], wg_d.ap()[k * 128:(k + 1) * 128, :], ("wg", k))
        load_w_cast(C, wb[:, k, :], wb_d.ap()[k * 128:(k + 1) * 128, :], ("wb", k))
        load_w_cast(C, wo[:, k, :], wo_d.ap()[k * 128:(k + 1) * 128, :], ("wo", k))
    xbs = [C.sb("xb", [128, NK, TB], F32) for _ in range(2)]
    hT = C.sb("hT", [128, NK, TB], BF16)
    yT = C.sb("yT", [128, NK, TB], BF16)
    mT = C.sb("mT", [128, NK, TB], BF16)
    sq = [C.sb("sq", [128, TB], BF16) for _ in range(2)]
    rs = (C.sb("lnv", [128, TB], F32), C.sb("rstd", [128, TB], F32))
    sg = [C.sb("sg", [128, TB], F32) for _ in range(2)]
    tp = [C.sb("tp", [128, TB], F32) for _ in range(3)]
    macc = C.sb("macc", [128, TB], F32)
    branch_k = [(0, 2), (2, 6), (6, 8)]
    for i in range(3):
        P.op(C.dq, lambda e, i=i: e.dma_start(out=yL[i].ap(), in_=yG[i].ap()[:, bass.ds(C.P.hid(e) * T, T)]),
             w=[("yL", i)], dma=True)

    def load_x(b):
        xb = xbs[b % 2]
        P.op("sp", lambda e, xb=xb, b=b: e.dma_start(out=xb[:], in_=xblk(x_src, b)), r=[("xd", b)], w=[("xb", b % 2)], dma=True)

    def do_block(b):
        xb = xbs[b % 2]
        xkey = ("xb", b % 2)
        if b + 1 < nb:
            load_x(b + 1)
        for i, (k0, k1) in enumerate(branch_k):
            P.op("sp", lambda e, b=b, k0=k0, k1=k1, i=i: e.dma_start(
                out=yT[:, k0:k1, :],
                in_=yL[i].ap().rearrange("(k p) t -> p k t", p=128)[:, :, b * TB:(b + 1) * TB]),
                r=[("yL", i)], w=[("yT", k) for k in range(k0, k1)], dma=True)
        emit_rms(C, xb, xkey, ones, gcol, lambda k: (hT[:, k, :], [("hT", k)]), sq, rs, 0, "m")
        it = 0
        for c in range(NK):
            for j in range(3):
                bp, bg = 1 + (it % 2), 3 + (it % 2)
                k0, k1 = branch_k[j]
                for k in range(k0, k1):
                    P.op("pe", lambda e, k=k, c=c, bp=bp, k0=k0, k1=k1: e.matmul(
                        C.pb[bp][:], lhsT=wb[:, k, c * 128:(c + 1) * 128], rhs=yT[:, k, :], start=(k == k0), stop=(k == k1 - 1)),
                        r=[("yT", k), ("wb", k)], w=[("pb", bp)])
                for k in range(NK):
                    P.op("pe", lambda e, k=k, c=c, j=j, bg=bg: e.matmul(
                        C.pb[bg][:], lhsT=wg[:, k, j * D + c * 128:j * D + (c + 1) * 128], rhs=hT[:, k, :],
                        start=(k == 0), stop=(k == NK - 1)), r=[("hT", k), ("wg", k)], w=[("pb", bg)])
                s = sg[it % 2]
                P.op("act", lambda e, s=s, bg=bg: e.activation(out=s[:], in_=C.pb[bg][:], func=AF.Sigmoid),
                     r=[("pb", bg)], w=[("sg", it % 2)])
                P.op("dve", lambda e, s=s, bp=bp, j=j: e.tensor_tensor(out=tp[j][:], in0=s[:], in1=C.pb[bp][:], op=ALU.mult),
                     r=[("sg", it % 2), ("pb", bp)], w=[("tp", j)])
                it += 1
            P.op("pool", lambda e: e.tensor_tensor(out=macc[:], in0=tp[0][:], in1=tp[1][:], op=ALU.add),
                 r=[("tp", 0), ("tp", 1)], w=["macc"])
            P.op("pool", lambda e, c=c: e.tensor_tensor(out=mT[:, c, :], in0=macc[:], in1=tp[2][:], op=ALU.add),
                 r=["macc", ("tp", 2)], w=[("mT", c)])
        for c in range(NK):
            py, ky = C.pb[5 + (c % 2)], ("pb", 5 + (c % 2))
            for k in range(NK):
                P.op("pe", lambda e, c=c, k=k, py=py: e.matmul(py[:], lhsT=wo[:, k, c * 128:(c + 1) * 128], rhs=mT[:, k, :],
                                                               start=(k == 0), stop=(k == NK - 1)),
                     r=[("mT", k), ("wo", k)], w=[ky])
            P.op("dve", lambda e, c=c, py=py: e.tensor_tensor(out=xb[:, c, :], in0=py[:], in1=xb[:, c, :], op=ALU.add),
                 r=[ky, xkey], w=[xkey])
        P.op("sp", lambda e, b=b, xb=xb: e.dma_start(out=xblk(x_dst, b), in_=xb[:]), r=[xkey], w=[("xd", b)], dma=True)

    load_x(0)
    for b in range(nb):
        do_block(b)


def load_fm(C, dst, fmG, S, which, key):
    gv = fmG.ap().rearrange("(j r p) t -> j p r t", r=4, j=8)
    jj, p0 = which // 2, (which % 2) * 64
    C.P.op(C.dq, lambda e: e.dma_start(out=dst[:, :].rearrange("p (r t) -> p r t", r=4),
                                       in_=gv[bass.ds(C.P.hid(e) * 2 + jj, 1), p0:p0 + 64, :, :]),
           r=[("fmG", 2 * h + jj) for h in range(4)], w=[key], dma=True)


def load_tm(C, dst2d, tmG, S, g, off, w, key):
    tv = tmG.ap().rearrange("(h g r p) x -> h g p r x", h=4, g=3, r=4)
    C.P.op(C.dq, lambda e: e.dma_start(out=dst2d[:, :].rearrange("p (r x) -> p r x", r=4),
                                       in_=tv[bass.ds(C.P.hid(e), 1), g, :, :, off:off + w]),
           r=[("tmG", g, h) for h in range(4)], w=[key], dma=True)


def emit_sb(C, S, fmG, tmG, cst_d, ysb_out):
    P = C.P
    C.reset()
    NB = S // 128
    NG = S // TB
    qT = C.sb("qT", [64, S], BF16)
    kT = C.sb("kT", [64, S], BF16)
    V2 = C.sb("V", [128, NB * 64], BF16)
    V = V2[:, :].rearrange("p (n d) -> p n d", d=64)
    tri = C.sb("tri", [128, 128], BF16)
    tric = C.sb("tric", [128, 128], BF16)
    onesm = C.sb("onesm", [128, 128], BF16)
    mk = C.sb("mk", [128, 4, TB], BF16)
    load_fm(C, qT, fmG, S, 0, "qT")
    load_fm(C, kT, fmG, S, 1, "kT")
    load_tm(C, V2, tmG, S, 0, 0, (S // 512) * 64, "V")
    P.op("sp", lambda e: e.dma_start(out=tri[:], in_=cst_d["tri"].ap()), w=["tri"], dma=True)
    P.op("sp", lambda e: e.dma_start(out=tric[:], in_=cst_d["tric"].ap()), w=["tric"], dma=True)
    P.op("sp", lambda e: e.dma_start(out=mk[:], in_=cst_d["mask"].ap().rearrange("p (r t) -> p r t", t=TB)), w=["mk"], dma=True)
    P.op("pool", lambda e: e.memset(onesm[:], 1.0), w=["onesm"])
    E = [C.sb("E", [128, 2, TB], F32) for _ in range(3)]
    L = [C.sb("L", [128, 2, TB], BF16) for _ in range(3)]
    W = [C.sb("W", [128, 2, TB], BF16) for _ in range(2)]
    ost = [C.sb("ost", [64, TB], BF16) for _ in range(2)]
    pz = [C.pb[0], C.pb[1]]
    pA = [C.pb[2], C.pb[3]]
    pX = [C.pb[4], C.pb[5]]
    z2, A2, X2 = C.pb2[0], C.pb2[1], C.pb2[2]
    kz, kA, kX = "pz", "pA", "pX"

    def do_group(g, it):
        npair = 2 * g + 2
        pO, kO = C.pb[6 + (g % 2)], ("pb", 6 + (g % 2))
        qs = qT[:, g * TB:(g + 1) * TB]

        def stage1(pi, i):
            j0 = 4 * g + 3 - 2 * pi
            Ei, Li = E[i % 3], L[i % 3]
            for u in range(2):
                jb = j0 - u
                P.op("pe", lambda e, u=u, jb=jb: e.matmul(pz[u][:], lhsT=kT[:, jb * 128:(jb + 1) * 128], rhs=qs, start=True, stop=True),
                     r=["qT", "kT"], w=[kz])
            P.op("act", lambda e: e.activation(out=Ei[:].rearrange("p u t -> p (u t)"), in_=z2[:], func=AF.Exp),
                 r=[kz], w=[("E", i % 3)])
            if pi < 2:
                P.op("dve", lambda e: e.tensor_tensor(out=Ei[:], in0=Ei[:], in1=mk[:, 2 * pi:2 * pi + 2, :], op=ALU.mult),
                     r=[("E", i % 3), "mk"], w=[("E", i % 3)])
            P.op("act", lambda e: e.activation(out=Li[:], in_=Ei[:], func=AF.Ln, bias=1.0, scale=1.0),
                 r=[("E", i % 3)], w=[("L", i % 3)])

        def mm(i, bank, m, u, st):
            Li = L[i % 3]
            P.op("pe", lambda e: e.matmul(bank[:], lhsT=m[:], rhs=Li[:, u, :], start=st, stop=False, skip_group_check=True),
                 r=[("L", i % 3), "tri", "tric", "onesm"], w=[kA])

        def s2a(pi, i):
            first = (pi == 0)
            mm(i, pA[0], tri, 0, first)
            mm(i, pA[1], onesm, 0, first)
            mm(i, pA[1], tri, 1, False)
            P.op("act", lambda e: e.activation(out=X2[:], in_=A2[:], func=AF.Exp, scale=-1.0), r=[kA], w=[kX])

        def s2b(pi, i):
            Ei, Wi = E[i % 3], W[i % 2]
            if pi != npair - 1:
                mm(i, pA[0], tric, 0, False)
                mm(i, pA[0], onesm, 1, False)
                mm(i, pA[1], tric, 1, False)
            P.op("dve", lambda e: e.tensor_tensor(out=Wi[:].rearrange("p u t -> p (u t)"), in0=Ei[:].rearrange("p u t -> p (u t)"),
                                                  in1=X2[:], op=ALU.mult),
                 r=[("E", i % 3), kX], w=[("W", i % 2)])

        def s2c(pi, i):
            j0 = 4 * g + 3 - 2 * pi
            Wi = W[i % 2]
            for u in range(2):
                jb = j0 - u
                P.op("pe", lambda e, u=u, jb=jb: e.matmul(pO[0:64, :], lhsT=V[:, jb, :], rhs=Wi[:, u, :],
                                                          start=(pi == 0 and u == 0), stop=(pi == npair - 1 and u == 1)),
                     r=[("W", i % 2), "V"], w=[kO])

        stage1(0, it)
        stage1(1, it + 1)
        for pi in range(npair):
            s2a(pi, it + pi)
            if pi + 2 < npair:
                stage1(pi + 2, it + pi + 2)
            s2b(pi, it + pi)
            if pi > 0:
                s2c(pi - 1, it + pi - 1)
        s2c(npair - 1, it + npair - 1)
        o = ost[g % 2]
        P.op("act", lambda e, o=o, pO=pO: e.activation(out=o[:], in_=pO[0:64, :], func=AF.Copy), r=[kO], w=[("ost", g % 2)])
        P.op("sp", lambda e, o=o, g=g: e.dma_start(out=ysb_out.ap()[:, g * TB:(g + 1) * TB], in_=o[:]),
             r=[("ost", g % 2)], dma=True)
        return it + npair

    it = 0
    for g in range(NG):
        it = do_group(g, it)


def emit_ret(C, S, fmG, tmG, cst_d, yret_out):
    P = C.P
    C.reset()
    N = S // 128
    qT = C.sb("qT", [64, S], BF16)
    kT = C.sb("kT", [64, S], BF16)
    nl = S // 4 // 128
    V2 = C.sb("V", [128, N * 128], BF16)
    G2 = C.sb("G", [128, N * 128], BF16)
    V = V2[:, :].rearrange("p (n d) -> p n d", d=128)
    G = G2[:, :].rearrange("p (n d) -> p n d", d=128)
    stb = C.sb("stb", [64, N, 128], BF16)
    st = C.sb("st", [64, 128], F32)
    dec = C.sb("dec", [128, 128], F32)
    qdec = C.sb("qdec", [64, 128], F32)
    kdec = C.sb("kdec", [128, 1], F32)
    cdv = C.sb("cdv", [64, 1], F32)
    idn = C.sb("idn", [128, 128], BF16)
    load_fm(C, qT, fmG, S, 2, "qT")
    load_fm(C, kT, fmG, S, 3, "kT")
    load_tm(C, V2, tmG, S, 1, 0, nl * 128, "V")
    load_tm(C, G2, tmG, S, 2, 0, nl * 128, "G")
    for nm, t in (("dec", dec), ("qdec", qdec), ("kdec", kdec), ("cdv", cdv), ("idn", idn)):
        P.op("sp", lambda e, nm=nm, t=t: e.dma_start(out=t[:], in_=cst_d[nm].ap()), w=[nm], dma=True)
    P.op("dve", lambda e: e.memset(st[:], 0.0), w=["st"])
    Kd = [C.sb("Kd", [128, 64], BF16) for _ in range(2)]
    for n in range(N):
        i = n % 2
        P.op("dve", lambda e, n=n: e.tensor_copy(out=stb[:, n, :], in_=st[:]), r=["st"], w=[("stb", n)])
        if n == N - 1:
            break
        ptr, ktr = C.pbh[i], ("pb", i)
        P.op("pe", lambda e, n=n, ptr=ptr: e.transpose(out=ptr[:, 0:64], in_=kT[:, n * 128:(n + 1) * 128], identity=idn[0:64, 0:64]),
             r=["kT", "idn"], w=[ktr])
        P.op("dve", lambda e, i=i, ptr=ptr: e.tensor_scalar(out=Kd[i][:], in0=ptr[:, 0:64], scalar1=kdec[:, 0:1], scalar2=None,
                                                            op0=ALU.mult), r=[ktr, "kdec"], w=[("Kd", i)])
        pkv, kkv = C.pb[2 + i], ("pb", 2 + i)
        P.op("pe", lambda e, n=n, i=i, pkv=pkv: e.matmul(pkv[0:64, 0:128], lhsT=Kd[i][:], rhs=V[:, n, :], start=True, stop=True),
             r=[("Kd", i), "V"], w=[kkv])
        P.op("dve", lambda e, pkv=pkv: e.scalar_tensor_tensor(out=st[:], in0=st[:], scalar=cdv[:, 0:1], in1=pkv[0:64, 0:128],
                                                              op0=ALU.mult, op1=ALU.add), r=["st", kkv, "cdv"], w=["st"])
    Sm = [C.sb("Sm", [128, 128], BF16) for _ in range(2)]
    Qd = [C.sb("Qd", [64, 128], BF16) for _ in range(2)]
    ysq = [C.sb("ysq", [128, 128], F32) for _ in range(2)]
    ss = [C.sb("ss", [128, 1], F32) for _ in range(2)]
    lr = [C.sb("lr", [128, 1], F32) for _ in range(2)]
    rr = [C.sb("rr", [128, 1], F32) for _ in range(2)]
    yo = [C.sb("yo", [128, 128], BF16) for _ in range(2)]
    yst = [C.sb("yst", [128, TB], BF16) for _ in range(2)]
    for n in range(N):
        i = n % 2
        psc, ksc = C.pb[4 + i], ("pb", 4 + i)
        P.op("pe", lambda e, n=n, psc=psc: e.matmul(psc[:, 0:128], lhsT=kT[:, n * 128:(n + 1) * 128], rhs=qT[:, n * 128:(n + 1) * 128],
                                                    start=True, stop=True), r=["kT", "qT"], w=[ksc])
        P.op("dve", lambda e, i=i, psc=psc: e.tensor_tensor(out=Sm[i][:], in0=psc[:, 0:128], in1=dec[:], op=ALU.mult),
             r=[ksc, "dec"], w=[("Sm", i)])
        P.op("pool", lambda e, i=i, n=n: e.tensor_tensor(out=Qd[i][:], in0=qT[:, n * 128:(n + 1) * 128], in1=qdec[:], op=ALU.mult),
             r=["qT", "qdec"], w=[("Qd", i)])
        py, ky = C.pb[6 + i], ("pb", 6 + i)
        P.op("pe", lambda e, i=i, n=n, py=py: e.matmul(py[:, 0:128], lhsT=Sm[i][:], rhs=V[:, n, :], start=True, stop=False),
             r=[("Sm", i), "V"], w=[ky])
        P.op("pe", lambda e, i=i, n=n, py=py: e.matmul(py[:, 0:128], lhsT=Qd[i][:], rhs=stb[:, n, :], start=False, stop=True),
             r=[("Qd", i), ("stb", n)], w=[ky])
        P.op("act", lambda e, i=i, py=py: e.activation(out=ysq[i][:], in_=py[:, 0:128], func=AF.Square, accum_out=ss[i][:]),
             r=[ky], w=[("ysq", i), ("ss", i)])
        P.op("act", lambda e, i=i: e.activation(out=lr[i][:], in_=ss[i][:], func=AF.Ln, bias=EPS, scale=1.0 / 128),
             r=[("ss", i)], w=[("lr", i)])
        P.op("act", lambda e, i=i: e.activation(out=rr[i][:], in_=lr[i][:], func=AF.Exp, scale=-0.5),
             r=[("lr", i)], w=[("rr", i)])
        P.op("dve", lambda e, i=i, n=n, py=py: e.scalar_tensor_tensor(out=yo[i][:], in0=py[:, 0:128], scalar=rr[i][:, 0:1], in1=G[:, n, :],
                                                               op0=ALU.mult, op1=ALU.mult), r=[ky, ("rr", i), "G"], w=[("yo", i)])
        pt, kt = C.pbh[i], ("pb", i)
        P.op("pe", lambda e, i=i, pt=pt: e.transpose(out=pt[:, 0:128], in_=yo[i][:], identity=idn[:]), r=[("yo", i), "idn"], w=[kt])
        gi = (n // 4) % 2
        P.op("act", lambda e, n=n, gi=gi, pt=pt: e.activation(out=yst[gi][:, (n % 4) * 128:(n % 4 + 1) * 128], in_=pt[:, 0:128], func=AF.Copy),
             r=[kt], w=[("yst", gi)])
        if n % 4 == 3:
            P.op("sp", lambda e, n=n, gi=gi: e.dma_start(out=yret_out.ap()[:, (n - 3) * 128:(n + 1) * 128], in_=yst[gi][:]),
                 r=[("yst", gi)], dma=True)


def emit_pool(C, S, tmG, wp_d, ps_d, cst_d, ypool_out):
    P = C.P
    C.reset()
    N = S // 128
    nl = S // 4 // 128
    U2 = C.sb("U", [128, N * 64], BF16)
    U = U2[:, :].rearrange("p (n d) -> p n d", d=64)
    wp = C.sb("wp", [64, 64], BF16)
    psc = C.sb("psc", [64, 1], F32)
    b0 = C.sb("b0", [128, 128], BF16)
    b0f = C.sb("b0f", [128, 128], BF16)
    b1 = C.sb("b1", [128, 128], BF16)
    load_tm(C, U2, tmG, S, 0, nl * 64, nl * 64, "U")
    P.op("pool", lambda e: e.dma_start(out=wp[:], in_=wp_d.ap()), w=["wp"], dma=True)
    P.op("sp", lambda e: e.dma_start(out=psc[:], in_=ps_d.ap()), w=["psc"], dma=True)
    for nm, t in (("b0", b0), ("b0f", b0f), ("b1", b1)):
        P.op("sp", lambda e, nm=nm, t=t: e.dma_start(out=t[:], in_=cst_d[nm].ap()), w=[nm], dma=True)
    zb = [C.sb("zb", [64, TB], BF16) for _ in range(2)]
    yp = [C.sb("yp", [64, TB], BF16) for _ in range(2)]
    for g in range(S // TB):
        i = g % 2
        pz, kz = C.pb[i], ("pb", i)
        for s in range(4):
            n = 4 * g + s
            cur = b0f if n == 0 else b0
            P.op("pe", lambda e, n=n, s=s, cur=cur, pz=pz: e.matmul(pz[0:64, s * 128:(s + 1) * 128], lhsT=U[:, n, :], rhs=cur[:],
                                                                    start=True, stop=(n == 0)), r=["U", "b0", "b0f"], w=[kz])
            if n > 0:
                P.op("pe", lambda e, n=n, s=s, pz=pz: e.matmul(pz[0:64, s * 128:(s + 1) * 128], lhsT=U[:, n - 1, :], rhs=b1[:],
                                                               start=False, stop=True), r=["U", "b1"], w=[kz])
        P.op("dve", lambda e, i=i, pz=pz: e.tensor_copy(out=zb[i][:], in_=pz[0:64, :]), r=[kz], w=[("zb", i)])
        py, ky = C.pb[2 + i], ("pb", 2 + i)
        P.op("pe", lambda e, i=i, py=py: e.matmul(py[0:64, :], lhsT=wp[:], rhs=zb[i][:], start=True, stop=True),
             r=[("zb", i), "wp"], w=[ky])
        P.op("dve", lambda e, i=i, py=py: e.tensor_scalar(out=yp[i][:], in0=py[0:64, :], scalar1=psc[:, 0:1], scalar2=None, op0=ALU.mult),
             r=[ky, "psc"], w=[("yp", i)])
        P.op("sp", lambda e, i=i, g=g: e.dma_start(out=ypool_out.ap()[:, g * TB:(g + 1) * TB], in_=yp[i][:]), r=[("yp", i)], dma=True)


def rope_tables(S, T, j):
    half = 32
    pos = np.arange(j * T, (j + 1) * T, dtype=np.float32)
    inv_freq = (np.float32(10000.0) ** (-np.arange(half, dtype=np.float32) / np.float32(half))).astype(np.float32)
    ang = (pos[:, None] * inv_freq[None, :]).astype(np.float32)
    c = np.cos(ang).astype(np.float32).T
    s = np.sin(ang).astype(np.float32).T
    cos64 = np.concatenate([c, c], 0)
    sin64 = np.concatenate([-s, s], 0)
    cos = np.concatenate([cos64, cos64 * 0.125], 0)
    sin = np.concatenate([sin64, sin64 * 0.125], 0)
    return np.ascontiguousarray(cos, np.float32), np.ascontiguousarray(sin, np.float32)


def mixer_consts(h):
    j = np.arange(128)
    tri = (j[:, None] >= j[None, :]).astype(np.float32)
    tric = 1.0 - tri
    c = np.arange(TB)
    mask = np.stack([(c[None, :] > (128 * r + j[:, None])).astype(np.float32) for r in (3, 2, 1, 0)], 1)
    lg = np.log(np.float32(1.0) - np.float32(2.0) ** np.float32(-5.0 - h)).astype(np.float32)
    idx = np.arange(128, dtype=np.float32)
    diff = idx[None, :] - idx[:, None]
    dec = np.where(diff >= 0, np.exp(np.where(diff >= 0, diff, 0.0) * lg), 0.0).astype(np.float32)
    qdec = np.tile(np.exp((idx + 1) * lg)[None, :], (64, 1)).astype(np.float32)
    kdec = np.exp((127 - idx) * lg)[:, None].astype(np.float32)
    cdv = np.full((64, 1), np.exp(128 * lg), np.float32)
    w = POOL_WINDOWS[h]
    t = np.arange(128)
    s_ = np.arange(128)
    inwin = ((s_[:, None] <= t[None, :]) & (s_[:, None] > t[None, :] - w)).astype(np.float32)
    eye = np.eye(128, dtype=np.float32)
    b0 = inwin / w - eye
    cnt = np.minimum(t + 1, w).astype(np.float32)
    b0f = inwin / cnt[None, :] - eye
    b1 = (((s_[:, None] - 128) > (t[None, :] - w))).astype(np.float32) / w
    return dict(tri=tri.astype(NPBF), tric=tric.astype(NPBF), mask=mask.reshape(128, 4 * TB).astype(NPBF),
                dec=dec, qdec=qdec, kdec=kdec, cdv=cdv, idn=np.eye(128, dtype=np.float32).astype(NPBF),
                b0=b0.astype(NPBF), b0f=b0f.astype(NPBF), b1=b1.astype(NPBF))


CONST_SPECS = dict(tri=([128, 128], BF16), tric=([128, 128], BF16), mask=([128, 4 * TB], BF16), dec=([128, 128], F32),
                   qdec=([64, 128], F32), kdec=([128, 1], F32), cdv=([64, 1], F32), idn=([128, 128], BF16),
                   b0=([128, 128], BF16), b0f=([128, 128], BF16), b1=([128, 128], BF16))


def gcol_layout(g):
    return np.ascontiguousarray(g.reshape(NK, 128).T, np.float32)


def perm_w_in(w):
    o = {"q_sb": 0, "k_sb": 256, "v_sb": 512, "q_r": 768, "k_r": 1024, "v_r": 1280, "g_r": 1792, "u_p": 2304, "gate": 2560}
    cols = []
    for h in range(4):
        cols += list(range(o["q_sb"] + 64 * h, o["q_sb"] + 64 * h + 64))
        cols += list(range(o["k_sb"] + 64 * h, o["k_sb"] + 64 * h + 64))
        cols += list(range(o["q_r"] + 64 * h, o["q_r"] + 64 * h + 64))
        cols += list(range(o["k_r"] + 64 * h, o["k_r"] + 64 * h + 64))
        for base in (o["q_r"], o["k_r"]):
            cols += list(range(base + 64 * h + 32, base + 64 * h + 64))
            cols += list(range(base + 64 * h, base + 64 * h + 32))
    cols += list(range(o["v_sb"], o["v_sb"] + 256))
    cols += list(range(o["u_p"], o["u_p"] + 256))
    cols += list(range(o["v_r"], o["v_r"] + 512))
    cols += list(range(o["g_r"], o["g_r"] + 512))
    assert len(cols) == 3072
    return np.ascontiguousarray(w[:, cols]), np.ascontiguousarray(w[:, o["gate"]:])


GROUPS = [[0, 1, 2, 3], [4, 5, 6, 7]]
CC_MAX_ELEMS = 524288


def y_chunk_rows(rows, S):
    return min(rows, max(1, CC_MAX_ELEMS // S))


def allgather(C, src, dst, r0, nrows, rkeys, wkeys):
    C.P.op("pool", lambda e: e.collective_compute("AllGather", ALU.bypass, replica_groups=GROUPS,
                                                 ins=[src.ap()[r0:r0 + nrows, :].opt()],
                                                 outs=[dst.ap()[4 * r0:4 * (r0 + nrows), :].opt()]),
           r=rkeys, w=wkeys, dma=True, cc=True)


def perm_branch_rows(w, S):
    rows = w.shape[0] // 4
    pr = y_chunk_rows(rows, S)
    idx = [r * rows + c * pr + p for c in range(rows // pr) for r in range(4) for p in range(pr)]
    return w[idx]


def gather_y(C, yb, yg, S):
    rows = yb.shape[0]
    pr = y_chunk_rows(rows, S)
    for c in range(rows // pr):
        allgather(C, yb, yg, c * pr, pr, [], [])


def build_program(S, depth, stop=99):
    T = S // 4
    nl = T // 128
    C = Ctx()
    x_in = C.din("x_in", [D, T], F32)
    x_out = C.dout("x_out", [D, T], F32)
    xs = C.dint("xs", [D, T], F32)
    cos = C.din("p_cos", [128, T], F32)
    sin = C.din("p_sin", [128, T], F32)
    scv = C.din("p_scv", [128, 1], F32)
    gfin = C.din("gfin", [128, NK], F32)
    cst = {k: C.din("c_" + k, shp, dt) for k, (shp, dt) in CONST_SPECS.items()}
    fmA = C.dint("fmA", [D, T], BF16)
    fmG = C.dint("fmG", [4 * D, T], BF16)
    tmA = C.dint("tmA", [1536, nl * 128], BF16)
    tmG = C.dint("tmG", [4 * 1536, nl * 128], BF16)
    nb = T // TB
    fmkeys = [("fmA", b) for b in range(nb)]
    yB = [C.dint("ysbB", [64, S], BF16), C.dint("yretB", [128, S], BF16), C.dint("ypoolB", [64, S], BF16)]
    yG = [C.dint("ysbG", [256, S], BF16), C.dint("yretG", [512, S], BF16), C.dint("ypoolG", [256, S], BF16)]
    yL = [C.dint("ysbL", [256, T], BF16), C.dint("yretL", [512, T], BF16), C.dint("ypoolL", [256, T], BF16)]
    cur = x_in
    for l in range(depth):
        C.dq = "sp" if l % 2 == 0 else "act"
        W = {n: C.din(f"{n}{l}", shp, F32) for n, shp in (
            ("f1g", [128, NK]), ("f1w1", [D, FF]), ("f1w3", [D, FF]), ("f1w2", [FF, D]),
            ("pg", [128, NK]), ("pwq", [D, 3072]), ("mwg", [D, 3072]), ("mwb", [D, D]), ("mwo", [D, D]),
            ("f2g", [128, NK]), ("f2w1", [D, FF]), ("f2w3", [D, FF]), ("f2w2", [FF, D]),
            ("wp", [64, 64]), ("psc", [64, 1]))}
        emit_ffn(C, T, cur, xs, W["f1g"], W["f1w1"], W["f1w3"], W["f1w2"], None)
        if stop <= 1: break
        emit_proj(C, T, xs, W["pg"], W["pwq"], cos, sin, scv, fmA, tmA)
        C.P.barrier()
        if stop <= 2: break
        for h in range(4):
            allgather(C, fmA, fmG, 2 * h * 128, 128, fmkeys, [("fmG", 2 * h)])
        for h in range(4):
            allgather(C, tmA, tmG, (h * 3) * 128, 128, ["tmA"], [("tmG", 0, h)])
        C.P.barrier()
        for h in range(4):
            allgather(C, fmA, fmG, (2 * h + 1) * 128, 128, fmkeys, [("fmG", 2 * h + 1)])
        for g in (1, 2):
            for h in range(4):
                allgather(C, tmA, tmG, (h * 3 + g) * 128, 128, ["tmA"], [("tmG", g, h)])
        if stop <= 3: break
        C.soft_reset = True
        emit_sb(C, S, fmG, tmG, cst, yB[0])
        if stop <= 4: break
        C.P.barrier()
        gather_y(C, yB[0], yG[0], S)
        C.skip_cc = True
        C.soft_reset = False
        emit_ret(C, S, fmG, tmG, cst, yB[1])
        if stop <= 5: break
        C.P.barrier(skip_cc=True)
        gather_y(C, yB[1], yG[1], S)
        emit_pool(C, S, tmG, W["wp"], W["psc"], cst, yB[2])
        C.skip_cc = False
        C.P.barrier()
        if stop <= 6: break
        gather_y(C, yB[2], yG[2], S)
        if stop <= 7: break
        emit_merge(C, T, xs, xs, W["pg"], W["mwg"], W["mwb"], W["mwo"], yG, yL)
        last = (l == depth - 1)
        emit_ffn(C, T, xs, x_out if last else xs, W["f2g"], W["f2w1"], W["f2w3"], W["f2w2"], gfin if last else None)
        cur = xs
    C.P.barrier()
    C.P.emit()
    return C


def forward(x, g_ffn1, w1_ffn1, w3_ffn1, w2_ffn1, g_mix, w_in, w_branch_sb, w_branch_ret, w_branch_pool, w_pool,
            pool_scale, w_out, g_ffn2, w1_ffn2, w3_ffn2, w2_ffn2, g_final):
    B, S, _ = x.shape
    depth = w_in.shape[0]
    assert B == 2
    T = S // 4
    f32 = lambda a: np.ascontiguousarray(np.asarray(a, np.float32))
    xf = f32(x).reshape(B * S, D)
    common = dict(p_scv=np.concatenate([np.full((64, 1), 0.125, np.float32), np.ones((64, 1), np.float32)], 0),
                  gfin=gcol_layout(f32(g_final)))
    for l in range(depth):
        wq, wg = perm_w_in(f32(w_in[l]))
        common.update({
            f"f1g{l}": gcol_layout(f32(g_ffn1[l])), f"f1w1{l}": f32(w1_ffn1[l]), f"f1w3{l}": f32(w3_ffn1[l]),
            f"f1w2{l}": f32(w2_ffn1[l]), f"pg{l}": gcol_layout(f32(g_mix[l])), f"pwq{l}": wq, f"mwg{l}": wg,
            f"mwb{l}": f32(np.concatenate([perm_branch_rows(f32(w_branch_sb[l]), S), perm_branch_rows(f32(w_branch_ret[l]), S),
                                           perm_branch_rows(f32(w_branch_pool[l]), S)], 0)),
            f"mwo{l}": f32(w_out[l]), f"f2g{l}": gcol_layout(f32(g_ffn2[l])), f"f2w1{l}": f32(w1_ffn2[l]),
            f"f2w3{l}": f32(w3_ffn2[l]), f"f2w2{l}": f32(w2_ffn2[l])})
    in_maps = []
    for c in range(NCORES):
        h = c % 4
        m = dict(common)
        m["x_in"] = np.ascontiguousarray(xf[c * T:(c + 1) * T].T)
        m["p_cos"], m["p_sin"] = rope_tables(S, T, c % 4)
        m.update({"c_" + k: v for k, v in mixer_consts(h).items()})
        for l in range(depth):
            m[f"wp{l}"] = f32(w_pool[l][h])
            m[f"psc{l}"] = f32(np.asarray(pool_scale[l], np.float32)[h * 64:(h + 1) * 64, None])
        in_maps.append(m)
    import os
    C = build_program(S, depth, int(os.environ.get("KSTOP", "99")))
    if os.environ.get("KTRACE"):
        rr = run_bass_kernel_spmd(C.nc, in_maps, core_ids=list(range(NCORES)), trace=True)
        print("KTRACE exec_time_ns", rr.exec_time_ns)
        res = rr.results
    else:
        res = run_bass_kernel_spmd(C.nc, in_maps, core_ids=list(range(NCORES))).results
    outf = np.concatenate([res[c]["x_out"].T for c in range(NCORES)], 0)
    return np.ascontiguousarray(outf.reshape(B, S, D).astype(np.float32))


def kernel(**inputs):
    inputs = {k: np.asarray(v) for k, v in inputs.items()}
    return forward(**inputs)
```

```python
import numpy as np
import ml_dtypes
import concourse.bass as bass
import concourse.mybir as mybir
from concourse.bass_utils import run_bass_kernel_spmd

F32 = mybir.dt.float32
BF16 = mybir.dt.bfloat16
AF = mybir.ActivationFunctionType
ALU = mybir.AluOpType
NPBF = ml_dtypes.bfloat16

D = 1024
FF = 2816
NK = 8
NF = 22
EPS = 1e-6
TB = 512
NCORES = 8
DEPTH = 2
BATCH = 2
POOL_WINDOWS = (2, 4, 8, 16)

SEM_EPOCH = 30000
N_DMA_SEMS = 6


class Prog:
    ENGS = ("pe", "act", "dve", "pool", "sp")

    def __init__(self, nc):
        self.nc = nc
        self.ops = []
        self.last_w = {}
        self.readers = {}
        self.n_sig = {e: 0 for e in self.ENGS}
        self.n_dma = {e: 0 for e in self.ENGS}
        self.n_cc = 0
        self.last_cc = None

    def op(self, eng, fn, r=(), w=(), dma=False, extra_deps=(), cc=False):
        deps = set(extra_deps)
        for k in r:
            if k in self.last_w:
                deps.add(self.last_w[k])
        for k in w:
            if k in self.last_w:
                deps.add(self.last_w[k])
            for v in self.readers.get(k, {}).values():
                deps.add(v)
        idx = len(self.ops)
        o = dict(eng=eng, fn=fn, deps=deps, dma=dma, idx=idx, cc=cc)
        if cc:
            self.n_cc += 1
            o["ccval"] = self.n_cc
            if self.last_cc is not None:
                deps.add(self.last_cc)
            self.last_cc = idx
        elif dma:
            n = self.n_dma[eng]
            self.n_dma[eng] = n + 1
            o["dslot"] = n % N_DMA_SEMS
            o["dval"] = 16 * (n // N_DMA_SEMS + 1)
            if n >= N_DMA_SEMS:
                o["prev_same_slot"] = True
        else:
            self.n_sig[eng] += 1
            o["sig"] = self.n_sig[eng]
        self.ops.append(o)
        for k in r:
            rk = ("dma", idx) if dma else eng
            self.readers.setdefault(k, {})[rk] = idx
        for k in w:
            self.last_w[k] = idx
            self.readers[k] = {}
        return idx

    def hid(self, e):
        k = id(e)
        if k not in self.hid_cache:
            self.hid_cache[k] = e.partition_id() % 4
        return self.hid_cache[k]

    def barrier(self, skip_cc=False):
        last = {}
        for o in self.ops:
            if o["cc"]:
                if not skip_cc:
                    last[("cc",)] = o["idx"]
            elif o["dma"]:
                last[("dma", o["eng"], o["dslot"])] = o["idx"]
            else:
                last[o["eng"]] = o["idx"]
        deps = set(last.values())
        for e in self.ENGS:
            self.op(e, None, extra_deps=deps)
        self.last_w = {}
        self.readers = {}

    def emit(self):
        nc = self.nc
        n_epochs = {e: (self.n_sig[e] // SEM_EPOCH) + 1 for e in self.ENGS}
        csem = {e: [nc.alloc_semaphore(f"c_{e}_{i}") for i in range(n_epochs[e])] for e in self.ENGS}
        dsem = {e: [nc.alloc_semaphore(f"d_{e}_{i}") for i in range(N_DMA_SEMS)]
                for e in self.ENGS if self.n_dma[e] > 0}
        ccsem = nc.alloc_semaphore("cc_sem") if self.n_cc else None

        def sig_of(o):
            if o["cc"]:
                return (("cc",), ccsem, o["ccval"])
            if o["dma"]:
                return (("d", o["eng"], o["dslot"]), dsem[o["eng"]][o["dslot"]], o["dval"])
            s = o["sig"]
            ep = (s - 1) // SEM_EPOCH
            return (("c", o["eng"], ep), csem[o["eng"]][ep], s - ep * SEM_EPOCH)

        per_eng = {e: [] for e in self.ENGS}
        for o in self.ops:
            per_eng[o["eng"]].append(o)
        ops = self.ops

        def build(ename):
            def body(e):
                self.hid_cache = {}
                waited = {}
                for o in per_eng[ename]:
                    waits = {}
                    for d in o["deps"]:
                        do = ops[d]
                        same = (not do["dma"]) and do["eng"] == ename
                        if same and ename == "pe" and not o["dma"]:
                            continue
                        if same and do["fn"] is None:
                            continue
                        key, sem, val = sig_of(do)
                        if waits.get(key, (None, 0))[1] < val:
                            waits[key] = (sem, val)
                    if o.get("prev_same_slot"):
                        key = ("d", ename, o["dslot"])
                        val = o["dval"] - 16
                        if waits.get(key, (None, 0))[1] < val:
                            waits[key] = (dsem[ename][o["dslot"]], val)
                    for key, (sem, val) in waits.items():
                        if waited.get(key, 0) >= val:
                            continue
                        waited[key] = val
                        e.wait_ge(sem, val)
                    ins = e.nop() if o["fn"] is None else o["fn"](e)
                    key, sem, val = sig_of(o)
                    if o["cc"]:
                        ins.then_inc(sem)
                    elif o["dma"]:
                        ins.then_inc(sem, 16)
                    else:
                        ins.then_inc(sem, 1)
            return body

        with nc.Block() as block:
            block.sync(build("sp"))
            block.tensor(build("pe"))
            block.scalar(build("act"))
            block.vector(build("dve"))
            block.gpsimd(build("pool"))


class Ctx:
    def __init__(self):
        self.nc = bass.Bass("TRN2", target_bir_lowering=False)
        self.P = Prog(self.nc)
        self.base = 16512
        self.top = 229344
        self.off = self.base
        self.uid = 0
        self.pb2 = [self.nc.alloc_psum_tensor(f"pb2_{i}", [128, 1024], F32) for i in range(4)]
        self.pb = [self.pb2[i // 2][:, (i % 2) * 512:(i % 2 + 1) * 512] for i in range(8)]
        self.pbh = [self.pb2[i // 2].bitcast(BF16)[:, (i % 2) * 1024:(i % 2) * 1024 + 1024] for i in range(8)]
        self.soft_reset = False
        self.skip_cc = False

    def reset(self):
        if self.soft_reset:
            self.soft_reset = False
        else:
            self.P.barrier(skip_cc=self.skip_cc)
        self.off = self.base

    def sb(self, name, shape, dtype):
        n = 1
        for s in shape[1:]:
            n *= s
        nbytes = n * (4 if dtype == F32 else 2)
        off = (self.off + 31) // 32 * 32
        assert off + nbytes <= self.top, (name, off, nbytes, self.top)
        self.off = off + nbytes
        self.uid += 1
        return self.nc.alloc_sbuf_tensor_at(f"{name}_{self.uid}", list(shape), dtype, offset=off)

    def din(self, name, shape, dtype):
        return self.nc.dram_tensor(name, list(shape), dtype, kind="ExternalInput")

    def dout(self, name, shape, dtype):
        return self.nc.dram_tensor(name, list(shape), dtype, kind="ExternalOutput")

    def dint(self, name, shape, dtype):
        return self.nc.dram_tensor(name, list(shape), dtype)


def xblk(x_d, b):
    return x_d.ap().rearrange("(k p) t -> p k t", p=128)[:, :, b * TB:(b + 1) * TB]


def emit_consts(C):
    P = C.P
    ones = C.sb("ones", [128, 128], BF16)
    P.op("pool", lambda e: e.memset(ones[:], 1.0), w=["ones"])
    return ones


def emit_rms(C, xb, xkey, ones, gcol, out_fn, sq, rs, stat_bank, tag):
    P = C.P
    pst = C.pb[stat_bank]
    for k in range(NK):
        s = sq[k % 2]
        sk = ("sq", k % 2)
        P.op("pool", lambda e, s=s, k=k: e.tensor_tensor(out=s[:], in0=xb[:, k, :], in1=xb[:, k, :], op=ALU.mult),
             r=[xkey], w=[sk])
        P.op("pe", lambda e, s=s, k=k: e.matmul(pst[:], lhsT=ones[:], rhs=s[:], start=(k == 0), stop=(k == NK - 1)),
             r=[sk, "ones"], w=[("pb", stat_bank)])
    lnv, rstd = rs
    P.op("act", lambda e: e.activation(out=lnv[:], in_=pst[:], func=AF.Ln, bias=EPS, scale=1.0 / D),
         r=[("pb", stat_bank)], w=["lnv"])
    P.op("act", lambda e: e.activation(out=rstd[:], in_=lnv[:], func=AF.Exp, scale=-0.5),
         r=["lnv"], w=["rstd"])
    for k in range(NK):
        oap, okeys = out_fn(k)
        P.op("dve", lambda e, k=k, oap=oap: e.scalar_tensor_tensor(out=oap, in0=xb[:, k, :], scalar=gcol[:, k:k + 1],
                                                                   in1=rstd[:], op0=ALU.mult, op1=ALU.mult),
             r=[xkey, "rstd", "gcol" + tag], w=okeys)


def load_gcol(C, g_d, tag):
    gcol = C.sb("gcol" + tag, [128, NK], F32)
    C.P.op("sp", lambda e: e.dma_start(out=gcol[:], in_=g_d.ap()), w=["gcol" + tag], dma=True)
    return gcol


def load_w_cast(C, dst_ap, src_ap, key):
    C.P.op("pool", lambda e: e.dma_start(out=dst_ap, in_=src_ap, max_dma_last_dim=4096), w=[key], dma=True)


def emit_ffn(C, T, x_src, x_dst, g_d, w1_d, w3_d, w2_d, gfin_d=None):
    P = C.P
    C.reset()
    nb = T // TB
    ones = emit_consts(C)
    gcol = load_gcol(C, g_d, "f")
    gfin = load_gcol(C, gfin_d, "fin") if gfin_d is not None else None
    w1s = C.sb("w1s", [128, NK, FF], BF16)
    w3s = C.sb("w3s", [128, NK, FF], BF16)
    w2s = C.sb("w2s", [128, NF, D], BF16)
    for k in range(NK):
        load_w_cast(C, w1s[:, k, :], w1_d.ap()[k * 128:(k + 1) * 128, :], ("w1", k))
        load_w_cast(C, w3s[:, k, :], w3_d.ap()[k * 128:(k + 1) * 128, :], ("w3", k))
    for f in range(NF):
        load_w_cast(C, w2s[:, f, :], w2_d.ap()[f * 128:(f + 1) * 128, :], ("w2", f))
    xbs = [C.sb("xb", [128, NK, TB], F32) for _ in range(2)]
    hT = C.sb("hT", [128, NK, TB], BF16)
    gT = C.sb("gT", [128, NF, TB], BF16)
    sq = [C.sb("sq", [128, TB], BF16) for _ in range(2)]
    rs = (C.sb("lnv", [128, TB], F32), C.sb("rstd", [128, TB], F32))
    sil = [C.sb("sil", [128, TB], F32) for _ in range(2)]
    w1keys = [("w1", k) for k in range(NK)]
    w3keys = [("w3", k) for k in range(NK)]

    def load_x(b):
        xb = xbs[b % 2]
        P.op("sp", lambda e, xb=xb, b=b: e.dma_start(out=xb[:], in_=xblk(x_src, b)), r=[("xd", b)], w=[("xb", b % 2)], dma=True)

    def do_block(b):
        xb = xbs[b % 2]
        xkey = ("xb", b % 2)
        if b + 1 < nb:
            load_x(b + 1)
        emit_rms(C, xb, xkey, ones, gcol, lambda k: (hT[:, k, :], [("hT", k)]), sq, rs, 0, "f")
        for f in range(NF):
            pa, pbb = C.pb[1 + (f % 2)], C.pb[3 + (f % 2)]
            ka, kb = ("pb", 1 + (f % 2)), ("pb", 3 + (f % 2))
            for k in range(NK):
                P.op("pe", lambda e, k=k, f=f, pa=pa: e.matmul(pa[:], lhsT=w1s[:, k, f * 128:(f + 1) * 128], rhs=hT[:, k, :],
                                                               start=(k == 0), stop=(k == NK - 1)),
                     r=[("hT", k), ("w1", k)], w=[ka])
            for k in range(NK):
                P.op("pe", lambda e, k=k, f=f, pbb=pbb: e.matmul(pbb[:], lhsT=w3s[:, k, f * 128:(f + 1) * 128], rhs=hT[:, k, :],
                                                                 start=(k == 0), stop=(k == NK - 1)),
                     r=[("hT", k), ("w3", k)], w=[kb])
            s = sil[f % 2]
            P.op("act", lambda e, s=s, pa=pa: e.activation(out=s[:], in_=pa[:], func=AF.Silu), r=[ka], w=[("sil", f % 2)])
            P.op("dve", lambda e, s=s, pbb=pbb, f=f: e.tensor_tensor(out=gT[:, f, :], in0=s[:], in1=pbb[:], op=ALU.mult),
                 r=[("sil", f % 2), kb], w=[("gT", f)])
        for c in range(NK):
            py, ky = C.pb[5 + (c % 2)], ("pb", 5 + (c % 2))
            for f in range(NF):
                P.op("pe", lambda e, c=c, f=f, py=py: e.matmul(py[:], lhsT=w2s[:, f, c * 128:(c + 1) * 128], rhs=gT[:, f, :],
                                                               start=(f == 0), stop=(f == NF - 1)),
                     r=[("gT", f), ("w2", f)], w=[ky])
            P.op("dve", lambda e, c=c, py=py: e.scalar_tensor_tensor(out=xb[:, c, :], in0=py[:], scalar=0.5, in1=xb[:, c, :],
                                                                      op0=ALU.mult, op1=ALU.add),
                 r=[ky, xkey], w=[xkey])
        if gfin is not None:
            emit_rms(C, xb, xkey, ones, gfin, lambda k: (xb[:, k, :], [xkey]), sq, rs, 0, "fin")
        P.op("sp", lambda e, b=b, xb=xb: e.dma_start(out=xblk(x_dst, b), in_=xb[:]), r=[xkey], w=[("xd", b)], dma=True)

    load_x(0)
    for b in range(nb):
        do_block(b)


def emit_proj(C, T, x_src, g_d, wq_d, cos_d, sin_d, scv_d, fm_out, tm_out):
    P = C.P
    C.reset()
    nb = T // TB
    ones = emit_consts(C)
    gcol = load_gcol(C, g_d, "p")
    scv = C.sb("scv", [128, 1], F32)
    P.op("sp", lambda e: e.dma_start(out=scv[:], in_=scv_d.ap()), w=["scv"], dma=True)
    wq = C.sb("wq", [128, NK, 3072], BF16)
    for k in range(NK):
        load_w_cast(C, wq[:, k, :], wq_d.ap()[k * 128:(k + 1) * 128, :], ("wq", k))
    xbs = [C.sb("xb", [128, NK, TB], F32) for _ in range(2)]
    hT = C.sb("hT", [128, NK, TB], BF16)
    sq = [C.sb("sq", [128, TB], BF16) for _ in range(2)]
    rs = (C.sb("lnv", [128, TB], F32), C.sb("rstd", [128, TB], F32))
    cs = C.sb("cos", [128, TB], F32)
    sn = C.sb("sin", [128, TB], F32)
    t1 = C.sb("t1", [128, TB], F32)
    t2 = C.sb("t2", [128, TB], F32)
    fms = C.sb("fms", [128, 8, TB], BF16)
    nl = T // 128
    tms = C.sb("tms", [128, 4, 3, nl * 128], BF16)
    hkeys = [("hT", k) for k in range(NK)]
    wkeys = [("wq", k) for k in range(NK)]

    def load_x(b):
        xb = xbs[b % 2]
        P.op("sp", lambda e, xb=xb, b=b: e.dma_start(out=xb[:], in_=xblk(x_src, b)), r=[("xd", b)], w=[("xb", b % 2)], dma=True)

    def do_block(b):
        xb = xbs[b % 2]
        xkey = ("xb", b % 2)
        if b + 1 < nb:
            load_x(b + 1)
        P.op("sp", lambda e, b=b: e.dma_start(out=cs[:], in_=cos_d.ap()[:, b * TB:(b + 1) * TB]), w=["cos"], dma=True)
        P.op("sp", lambda e, b=b: e.dma_start(out=sn[:], in_=sin_d.ap()[:, b * TB:(b + 1) * TB]), w=["sin"], dma=True)
        emit_rms(C, xb, xkey, ones, gcol, lambda k: (hT[:, k, :], [("hT", k)]), sq, rs, 0, "p")

        def fm_mm(j, bank):
            for k in range(NK):
                P.op("pe", lambda e, k=k: e.matmul(C.pb[bank][:], lhsT=wq[:, k, j * 128:(j + 1) * 128], rhs=hT[:, k, :],
                                                   start=(k == 0), stop=(k == NK - 1)),
                     r=[("hT", k), ("wq", k)], w=[("pb", bank)])

        for h in range(4):
            fm_mm(3 * h, 1)
            P.op("dve", lambda e, h=h: e.tensor_scalar(out=fms[:, 2 * h, :], in0=C.pb[1][:], scalar1=scv[:, 0:1], scalar2=None,
                                                       op0=ALU.mult),
                 r=[("pb", 1), "scv"], w=[("fms", 2 * h)])
            fm_mm(3 * h + 1, 2)
            fm_mm(3 * h + 2, 3)
            P.op("dve", lambda e: e.tensor_tensor(out=t1[:], in0=C.pb[2][:], in1=cs[:], op=ALU.mult),
                 r=[("pb", 2), "cos"], w=["t1"])
            P.op("dve", lambda e: e.tensor_tensor(out=t2[:], in0=C.pb[3][:], in1=sn[:], op=ALU.mult),
                 r=[("pb", 3), "sin"], w=["t2"])
            P.op("pool", lambda e, h=h: e.tensor_tensor(out=fms[:, 2 * h + 1, :], in0=t1[:], in1=t2[:], op=ALU.add),
                 r=["t1", "t2"], w=[("fms", 2 * h + 1)])
        P.op("sp", lambda e, b=b: e.dma_start(out=fm_out.ap().rearrange("(j p) t -> p j t", p=128)[:, :, b * TB:(b + 1) * TB], in_=fms[:]),
             r=[("fms", j) for j in range(8)], w=[("fmA", b)], dma=True)
        for s in range(4):
            for j in range(3):
                bank = 4 + j
                for k in range(NK):
                    P.op("pe", lambda e, k=k, s=s, j=j, bank=bank: e.matmul(
                        C.pb[bank][:], lhsT=hT[:, k, s * 128:(s + 1) * 128], rhs=wq[:, k, 1536 + j * 512:1536 + (j + 1) * 512],
                        start=(k == 0), stop=(k == NK - 1)), r=[("hT", k), ("wq", k)], w=[("pb", bank)])
            n_l = 4 * b + s
            P.op("dve", lambda e, n_l=n_l: e.tensor_copy(out=tms[:, :, 0, n_l * 64:(n_l + 1) * 64],
                                                         in_=C.pb[4][:, 0:256].rearrange("p (h d) -> p h d", h=4)),
                 r=[("pb", 4)], w=[("tms", 0)])
            P.op("dve", lambda e, n_l=n_l: e.tensor_copy(out=tms[:, :, 0, nl * 64 + n_l * 64:nl * 64 + (n_l + 1) * 64],
                                                         in_=C.pb[4][:, 256:512].rearrange("p (h d) -> p h d", h=4)),
                 r=[("pb", 4)], w=[("tms", 1)])
            P.op("act", lambda e, n_l=n_l: e.activation(out=tms[:, :, 1, n_l * 128:(n_l + 1) * 128],
                                                        in_=C.pb[5][:].rearrange("p (h d) -> p h d", h=4), func=AF.Copy),
                 r=[("pb", 5)], w=[("tms", 2)])
            P.op("act", lambda e, n_l=n_l: e.activation(out=tms[:, :, 2, n_l * 128:(n_l + 1) * 128],
                                                        in_=C.pb[6][:].rearrange("p (h d) -> p h d", h=4), func=AF.Silu),
                 r=[("pb", 6)], w=[("tms", 3)])

    load_x(0)
    for b in range(nb):
        do_block(b)
    P.op("sp", lambda e: e.dma_start(out=tm_out.ap().rearrange("(h g p) x -> p h g x", h=4, g=3), in_=tms[:]),
         r=[("tms", i) for i in range(4)], w=["tmA"], dma=True)


def emit_merge(C, T, x_src, x_dst, g_d, wg_d, wb_d, wo_d, yG, yL):
    P = C.P
    C.reset()
    nb = T // TB
    ones = emit_consts(C)
    gcol = load_gcol(C, g_d, "m")
    wg = C.sb("wg", [128, NK, 3072], BF16)
    wb = C.sb("wb", [128, NK, D], BF16)
    wo = C.sb("wo", [128, NK, D], BF16)
    for k in range(NK):
        load_w_cast(C, wg[:, k, :], wg_d.ap()[k * 128:(k + 1) * 128, :], ("wg", k))
        load_w_cast(C, wb[:, k, :], wb_d.ap()[k * 128:(k + 1) * 128, :], ("wb", k))
        load_w_cast(C, wo[:, k, :], wo_d.ap()[k * 128:(k + 1) * 128, :], ("wo", k))
    xbs = [C.sb("xb", [128, NK, TB], F32) for _ in range(2)]
    hT = C.sb("hT", [128, NK, TB], BF16)
    yT = C.sb("yT", [128, NK, TB], BF16)
    mT = C.sb("mT", [128, NK, TB], BF16)
    sq = [C.sb("sq", [128, TB], BF16) for _ in range(2)]
    rs = (C.sb("lnv", [128, TB], F32), C.sb("rstd", [128, TB], F32))
    sg = [C.sb("sg", [128, TB], F32) for _ in range(2)]
    tp = [C.sb("tp", [128, TB], F32) for _ in range(3)]
    macc = C.sb("macc", [128, TB], F32)
    branch_k = [(0, 2), (2, 6), (6, 8)]
    for i in range(3):
        P.op(C.dq, lambda e, i=i: e.dma_start(out=yL[i].ap(), in_=yG[i].ap()[:, bass.ds(C.P.hid(e) * T, T)]),
             w=[("yL", i)], dma=True)

    def load_x(b):
        xb = xbs[b % 2]
        P.op("sp", lambda e, xb=xb, b=b: e.dma_start(out=xb[:], in_=xblk(x_src, b)), r=[("xd", b)], w=[("xb", b % 2)], dma=True)

    def do_block(b):
        xb = xbs[b % 2]
        xkey = ("xb", b % 2)
        if b + 1 < nb:
            load_x(b + 1)
        for i, (k0, k1) in enumerate(branch_k):
            P.op("sp", lambda e, b=b, k0=k0, k1=k1, i=i: e.dma_start(
                out=yT[:, k0:k1, :],
                in_=yL[i].ap().rearrange("(k p) t -> p k t", p=128)[:, :, b * TB:(b + 1) * TB]),
                r=[("yL", i)], w=[("yT", k) for k in range(k0, k1)], dma=True)
        emit_rms(C, xb, xkey, ones, gcol, lambda k: (hT[:, k, :], [("hT", k)]), sq, rs, 0, "m")
        it = 0
        for c in range(NK):
            for j in range(3):
                bp, bg = 1 + (it % 2), 3 + (it % 2)
                k0, k1 = branch_k[j]
                for k in range(k0, k1):
                    P.op("pe", lambda e, k=k, c=c, bp=bp, k0=k0, k1=k1: e.matmul(
                        C.pb[bp][:], lhsT=wb[:, k, c * 128:(c + 1) * 128], rhs=yT[:, k, :], start=(k == k0), stop=(k == k1 - 1)),
                        r=[("yT", k), ("wb", k)], w=[("pb", bp)])
                for k in range(NK):
                    P.op("pe", lambda e, k=k, c=c, j=j, bg=bg: e.matmul(
                        C.pb[bg][:], lhsT=wg[:, k, j * D + c * 128:j * D + (c + 1) * 128], rhs=hT[:, k, :],
                        start=(k == 0), stop=(k == NK - 1)), r=[("hT", k), ("wg", k)], w=[("pb", bg)])
                s = sg[it % 2]
                P.op("act", lambda e, s=s, bg=bg: e.activation(out=s[:], in_=C.pb[bg][:], func=AF.Sigmoid),
                     r=[("pb", bg)], w=[("sg", it % 2)])
                P.op("dve", lambda e, s=s, bp=bp, j=j: e.tensor_tensor(out=tp[j][:], in0=s[:], in1=C.pb[bp][:], op=ALU.mult),
                     r=[("sg", it % 2), ("pb", bp)], w=[("tp", j)])
                it += 1
            P.op("pool", lambda e: e.tensor_tensor(out=macc[:], in0=tp[0][:], in1=tp[1][:], op=ALU.add),
                 r=[("tp", 0), ("tp", 1)], w=["macc"])
            P.op("pool", lambda e, c=c: e.tensor_tensor(out=mT[:, c, :], in0=macc[:], in1=tp[2][:], op=ALU.add),
                 r=["macc", ("tp", 2)], w=[("mT", c)])
        for c in range(NK):
            py, ky = C.pb[5 + (c % 2)], ("pb", 5 + (c % 2))
            for k in range(NK):
                P.op("pe", lambda e, c=c, k=k, py=py: e.matmul(py[:], lhsT=wo[:, k, c * 128:(c + 1) * 128], rhs=mT[:, k, :],
                                                               start=(k == 0), stop=(k == NK - 1)),
                     r=[("mT", k), ("wo", k)], w=[ky])
            P.op("dve", lambda e, c=c, py=py: e.tensor_tensor(out=xb[:, c, :], in0=py[:], in1=xb[:, c, :], op=ALU.add),
                 r=[ky, xkey], w=[xkey])
        P.op("sp", lambda e, b=b, xb=xb: e.dma_start(out=xblk(x_dst, b), in_=xb[:]), r=[xkey], w=[("xd", b)], dma=True)

    load_x(0)
    for b in range(nb):
        do_block(b)


def load_fm(C, dst, fmG, S, which, key):
    gv = fmG.ap().rearrange("(j r p) t -> j p r t", r=4, j=8)
    jj, p0 = which // 2, (which % 2) * 64
    C.P.op(C.dq, lambda e: e.dma_start(out=dst[:, :].rearrange("p (r t) -> p r t", r=4),
                                       in_=gv[bass.ds(C.P.hid(e) * 2 + jj, 1), p0:p0 + 64, :, :]),
           r=[("fmG", 2 * h + jj) for h in range(4)], w=[key], dma=True)


def load_tm(C, dst2d, tmG, S, g, off, w, key):
    tv = tmG.ap().rearrange("(h g r p) x -> h g p r x", h=4, g=3, r=4)
    C.P.op(C.dq, lambda e: e.dma_start(out=dst2d[:, :].rearrange("p (r x) -> p r x", r=4),
                                       in_=tv[bass.ds(C.P.hid(e), 1), g, :, :, off:off + w]),
           r=[("tmG", g, h) for h in range(4)], w=[key], dma=True)


def emit_sb(C, S, fmG, tmG, cst_d, ysb_out):
    P = C.P
    C.reset()
    NB = S // 128
    NG = S // TB
    qT = C.sb("qT", [64, S], BF16)
    kT = C.sb("kT", [64, S], BF16)
    V2 = C.sb("V", [128, NB * 64], BF16)
    V = V2[:, :].rearrange("p (n d) -> p n d", d=64)
    tri = C.sb("tri", [128, 128], BF16)
    tric = C.sb("tric", [128, 128], BF16)
    onesm = C.sb("onesm", [128, 128], BF16)
    mk = C.sb("mk", [128, 4, TB], BF16)
    load_fm(C, qT, fmG, S, 0, "qT")
    load_fm(C, kT, fmG, S, 1, "kT")
    load_tm(C, V2, tmG, S, 0, 0, (S // 512) * 64, "V")
    P.op("sp", lambda e: e.dma_start(out=tri[:], in_=cst_d["tri"].ap()), w=["tri"], dma=True)
    P.op("sp", lambda e: e.dma_start(out=tric[:], in_=cst_d["tric"].ap()), w=["tric"], dma=True)
    P.op("sp", lambda e: e.dma_start(out=mk[:], in_=cst_d["mask"].ap().rearrange("p (r t) -> p r t", t=TB)), w=["mk"], dma=True)
    P.op("pool", lambda e: e.memset(onesm[:], 1.0), w=["onesm"])
    E = [C.sb("E", [128, 2, TB], F32) for _ in range(3)]
    L = [C.sb("L", [128, 2, TB], BF16) for _ in range(3)]
    W = [C.sb("W", [128, 2, TB], BF16) for _ in range(2)]
    ost = [C.sb("ost", [64, TB], BF16) for _ in range(2)]
    pz = [C.pb[0], C.pb[1]]
    pA = [C.pb[2], C.pb[3]]
    pX = [C.pb[4], C.pb[5]]
    z2, A2, X2 = C.pb2[0], C.pb2[1], C.pb2[2]
    kz, kA, kX = "pz", "pA", "pX"

    def do_group(g, it):
        npair = 2 * g + 2
        pO, kO = C.pb[6 + (g % 2)], ("pb", 6 + (g % 2))
        qs = qT[:, g * TB:(g + 1) * TB]

        def stage1(pi, i):
            j0 = 4 * g + 3 - 2 * pi
            Ei, Li = E[i % 3], L[i % 3]
            for u in range(2):
                jb = j0 - u
                P.op("pe", lambda e, u=u, jb=jb: e.matmul(pz[u][:], lhsT=kT[:, jb * 128:(jb + 1) * 128], rhs=qs, start=True, stop=True),
                     r=["qT", "kT"], w=[kz])
            P.op("act", lambda e: e.activation(out=Ei[:].rearrange("p u t -> p (u t)"), in_=z2[:], func=AF.Exp),
                 r=[kz], w=[("E", i % 3)])
            if pi < 2:
                P.op("dve", lambda e: e.tensor_tensor(out=Ei[:], in0=Ei[:], in1=mk[:, 2 * pi:2 * pi + 2, :], op=ALU.mult),
                     r=[("E", i % 3), "mk"], w=[("E", i % 3)])
            P.op("act", lambda e: e.activation(out=Li[:], in_=Ei[:], func=AF.Ln, bias=1.0, scale=1.0),
                 r=[("E", i % 3)], w=[("L", i % 3)])

        def mm(i, bank, m, u, st):
            Li = L[i % 3]
            P.op("pe", lambda e: e.matmul(bank[:], lhsT=m[:], rhs=Li[:, u, :], start=st, stop=False, skip_group_check=True),
                 r=[("L", i % 3), "tri", "tric", "onesm"], w=[kA])

        def s2a(pi, i):
            first = (pi == 0)
            mm(i, pA[0], tri, 0, first)
            mm(i, pA[1], onesm, 0, first)
            mm(i, pA[1], tri, 1, False)
            P.op("act", lambda e: e.activation(out=X2[:], in_=A2[:], func=AF.Exp, scale=-1.0), r=[kA], w=[kX])

        def s2b(pi, i):
            Ei, Wi = E[i % 3], W[i % 2]
            if pi != npair - 1:
                mm(i, pA[0], tric, 0, False)
                mm(i, pA[0], onesm, 1, False)
                mm(i, pA[1], tric, 1, False)
            P.op("dve", lambda e: e.tensor_tensor(out=Wi[:].rearrange("p u t -> p (u t)"), in0=Ei[:].rearrange("p u t -> p (u t)"),
                                                  in1=X2[:], op=ALU.mult),
                 r=[("E", i % 3), kX], w=[("W", i % 2)])

        def s2c(pi, i):
            j0 = 4 * g + 3 - 2 * pi
            Wi = W[i % 2]
            for u in range(2):
                jb = j0 - u
                P.op("pe", lambda e, u=u, jb=jb: e.matmul(pO[0:64, :], lhsT=V[:, jb, :], rhs=Wi[:, u, :],
                                                          start=(pi == 0 and u == 0), stop=(pi == npair - 1 and u == 1)),
                     r=[("W", i % 2), "V"], w=[kO])

        stage1(0, it)
        stage1(1, it + 1)
        for pi in range(npair):
            s2a(pi, it + pi)
            if pi + 2 < npair:
                stage1(pi + 2, it + pi + 2)
            s2b(pi, it + pi)
            if pi > 0:
                s2c(pi - 1, it + pi - 1)
        s2c(npair - 1, it + npair - 1)
        o = ost[g % 2]
        P.op("act", lambda e, o=o, pO=pO: e.activation(out=o[:], in_=pO[0:64, :], func=AF.Copy), r=[kO], w=[("ost", g % 2)])
        P.op("sp", lambda e, o=o, g=g: e.dma_start(out=ysb_out.ap()[:, g * TB:(g + 1) * TB], in_=o[:]),
             r=[("ost", g % 2)], dma=True)
        return it + npair

    it = 0
    for g in range(NG):
        it = do_group(g, it)


def emit_ret(C, S, fmG, tmG, cst_d, yret_out):
    P = C.P
    C.reset()
    N = S // 128
    qT = C.sb("qT", [64, S], BF16)
    kT = C.sb("kT", [64, S], BF16)
    nl = S // 4 // 128
    V2 = C.sb("V", [128, N * 128], BF16)
    G2 = C.sb("G", [128, N * 128], BF16)
    V = V2[:, :].rearrange("p (n d) -> p n d", d=128)
    G = G2[:, :].rearrange("p (n d) -> p n d", d=128)
    stb = C.sb("stb", [64, N, 128], BF16)
    st = C.sb("st", [64, 128], F32)
    dec = C.sb("dec", [128, 128], F32)
    qdec = C.sb("qdec", [64, 128], F32)
    kdec = C.sb("kdec", [128, 1], F32)
    cdv = C.sb("cdv", [64, 1], F32)
    idn = C.sb("idn", [128, 128], BF16)
    load_fm(C, qT, fmG, S, 2, "qT")
    load_fm(C, kT, fmG, S, 3, "kT")
    load_tm(C, V2, tmG, S, 1, 0, nl * 128, "V")
    load_tm(C, G2, tmG, S, 2, 0, nl * 128, "G")
    for nm, t in (("dec", dec), ("qdec", qdec), ("kdec", kdec), ("cdv", cdv), ("idn", idn)):
        P.op("sp", lambda e, nm=nm, t=t: e.dma_start(out=t[:], in_=cst_d[nm].ap()), w=[nm], dma=True)
    P.op("dve", lambda e: e.memset(st[:], 0.0), w=["st"])
    Kd = [C.sb("Kd", [128, 64], BF16) for _ in range(2)]
    def kv_stage(n):
        i = n % 2
        ptr, ktr = C.pbh[i], ("pb", i)
        P.op("pe", lambda e: e.transpose(out=ptr[:, 0:64], in_=kT[:, n * 128:(n + 1) * 128], identity=idn[0:64, 0:64]),
             r=["kT", "idn"], w=[ktr])
        P.op("dve", lambda e: e.tensor_scalar(out=Kd[i][:], in0=ptr[:, 0:64], scalar1=kdec[:, 0:1], scalar2=None,
                                              op0=ALU.mult), r=[ktr, "kdec"], w=[("Kd", i)])
        pkv, kkv = C.pb[2 + i], ("pb", 2 + i)
        P.op("pe", lambda e: e.matmul(pkv[0:64, 0:128], lhsT=Kd[i][:], rhs=V[:, n, :], start=True, stop=True),
             r=[("Kd", i), "V"], w=[kkv])

    if N > 1:
        kv_stage(0)
    for n in range(N):
        i = n % 2
        P.op("dve", lambda e, n=n: e.tensor_copy(out=stb[:, n, :], in_=st[:]), r=["st"], w=[("stb", n)])
        if n == N - 1:
            break
        if n + 1 < N - 1:
            kv_stage(n + 1)
        pkv, kkv = C.pb[2 + i], ("pb", 2 + i)
        P.op("dve", lambda e, pkv=pkv: e.scalar_tensor_tensor(out=st[:], in0=st[:], scalar=cdv[:, 0:1], in1=pkv[0:64, 0:128],
                                                              op0=ALU.mult, op1=ALU.add), r=["st", kkv, "cdv"], w=["st"])
    Sm = [C.sb("Sm", [128, 128], BF16) for _ in range(2)]
    Qd = [C.sb("Qd", [64, 128], BF16) for _ in range(2)]
    ysq = [C.sb("ysq", [128, 128], F32) for _ in range(2)]
    ss = [C.sb("ss", [128, 1], F32) for _ in range(2)]
    lr = [C.sb("lr", [128, 1], F32) for _ in range(2)]
    rr = [C.sb("rr", [128, 1], F32) for _ in range(2)]
    yo = [C.sb("yo", [128, 128], BF16) for _ in range(2)]
    yst = [C.sb("yst", [128, TB], BF16) for _ in range(2)]
    for n in range(N):
        i = n % 2
        psc, ksc = C.pb[4 + i], ("pb", 4 + i)
        P.op("pe", lambda e, n=n, psc=psc: e.matmul(psc[:, 0:128], lhsT=kT[:, n * 128:(n + 1) * 128], rhs=qT[:, n * 128:(n + 1) * 128],
                                                    start=True, stop=True), r=["kT", "qT"], w=[ksc])
        P.op("dve", lambda e, i=i, psc=psc: e.tensor_tensor(out=Sm[i][:], in0=psc[:, 0:128], in1=dec[:], op=ALU.mult),
             r=[ksc, "dec"], w=[("Sm", i)])
        P.op("pool", lambda e, i=i, n=n: e.tensor_tensor(out=Qd[i][:], in0=qT[:, n * 128:(n + 1) * 128], in1=qdec[:], op=ALU.mult),
             r=["qT", "qdec"], w=[("Qd", i)])
        py, ky = C.pb[6 + i], ("pb", 6 + i)
        P.op("pe", lambda e, i=i, n=n, py=py: e.matmul(py[:, 0:128], lhsT=Sm[i][:], rhs=V[:, n, :], start=True, stop=False),
             r=[("Sm", i), "V"], w=[ky])
        P.op("pe", lambda e, i=i, n=n, py=py: e.matmul(py[:, 0:128], lhsT=Qd[i][:], rhs=stb[:, n, :], start=False, stop=True),
             r=[("Qd", i), ("stb", n)], w=[ky])
        P.op("act", lambda e, i=i, py=py: e.activation(out=ysq[i][:], in_=py[:, 0:128], func=AF.Square, accum_out=ss[i][:]),
             r=[ky], w=[("ysq", i), ("ss", i)])
        P.op("act", lambda e, i=i: e.activation(out=lr[i][:], in_=ss[i][:], func=AF.Ln, bias=EPS, scale=1.0 / 128),
             r=[("ss", i)], w=[("lr", i)])
        P.op("act", lambda e, i=i: e.activation(out=rr[i][:], in_=lr[i][:], func=AF.Exp, scale=-0.5),
             r=[("lr", i)], w=[("rr", i)])
        P.op("dve", lambda e, i=i, n=n, py=py: e.scalar_tensor_tensor(out=yo[i][:], in0=py[:, 0:128], scalar=rr[i][:, 0:1], in1=G[:, n, :],
                                                               op0=ALU.mult, op1=ALU.mult), r=[ky, ("rr", i), "G"], w=[("yo", i)])
        pt, kt = C.pbh[i], ("pb", i)
        P.op("pe", lambda e, i=i, pt=pt: e.transpose(out=pt[:, 0:128], in_=yo[i][:], identity=idn[:]), r=[("yo", i), "idn"], w=[kt])
        gi = (n // 4) % 2
        P.op("act", lambda e, n=n, gi=gi, pt=pt: e.activation(out=yst[gi][:, (n % 4) * 128:(n % 4 + 1) * 128], in_=pt[:, 0:128], func=AF.Copy),
             r=[kt], w=[("yst", gi)])
        if n % 4 == 3:
            P.op("sp", lambda e, n=n, gi=gi: e.dma_start(out=yret_out.ap()[:, (n - 3) * 128:(n + 1) * 128], in_=yst[gi][:]),
                 r=[("yst", gi)], dma=True)


def emit_pool(C, S, tmG, wp_d, ps_d, cst_d, ypool_out):
    P = C.P
    C.reset()
    N = S // 128
    nl = S // 4 // 128
    U2 = C.sb("U", [128, N * 64], BF16)
    U = U2[:, :].rearrange("p (n d) -> p n d", d=64)
    wp = C.sb("wp", [64, 64], BF16)
    psc = C.sb("psc", [64, 1], F32)
    b0 = C.sb("b0", [128, 128], BF16)
    b0f = C.sb("b0f", [128, 128], BF16)
    b1 = C.sb("b1", [128, 128], BF16)
    load_tm(C, U2, tmG, S, 0, nl * 64, nl * 64, "U")
    P.op("pool", lambda e: e.dma_start(out=wp[:], in_=wp_d.ap()), w=["wp"], dma=True)
    P.op("sp", lambda e: e.dma_start(out=psc[:], in_=ps_d.ap()), w=["psc"], dma=True)
    for nm, t in (("b0", b0), ("b0f", b0f), ("b1", b1)):
        P.op("sp", lambda e, nm=nm, t=t: e.dma_start(out=t[:], in_=cst_d[nm].ap()), w=[nm], dma=True)
    zb = [C.sb("zb", [64, TB], BF16) for _ in range(2)]
    yp = [C.sb("yp", [64, TB], BF16) for _ in range(2)]
    for g in range(S // TB):
        i = g % 2
        pz, kz = C.pb[i], ("pb", i)
        for s in range(4):
            n = 4 * g + s
            cur = b0f if n == 0 else b0
            P.op("pe", lambda e, n=n, s=s, cur=cur, pz=pz: e.matmul(pz[0:64, s * 128:(s + 1) * 128], lhsT=U[:, n, :], rhs=cur[:],
                                                                    start=True, stop=(n == 0)), r=["U", "b0", "b0f"], w=[kz])
            if n > 0:
                P.op("pe", lambda e, n=n, s=s, pz=pz: e.matmul(pz[0:64, s * 128:(s + 1) * 128], lhsT=U[:, n - 1, :], rhs=b1[:],
                                                               start=False, stop=True), r=["U", "b1"], w=[kz])
        P.op("dve", lambda e, i=i, pz=pz: e.tensor_copy(out=zb[i][:], in_=pz[0:64, :]), r=[kz], w=[("zb", i)])
        py, ky = C.pb[2 + i], ("pb", 2 + i)
        P.op("pe", lambda e, i=i, py=py: e.matmul(py[0:64, :], lhsT=wp[:], rhs=zb[i][:], start=True, stop=True),
             r=[("zb", i), "wp"], w=[ky])
        P.op("dve", lambda e, i=i, py=py: e.tensor_scalar(out=yp[i][:], in0=py[0:64, :], scalar1=psc[:, 0:1], scalar2=None, op0=ALU.mult),
             r=[ky, "psc"], w=[("yp", i)])
        P.op("sp", lambda e, i=i, g=g: e.dma_start(out=ypool_out.ap()[:, g * TB:(g + 1) * TB], in_=yp[i][:]), r=[("yp", i)], dma=True)


def rope_tables(S, T, j):
    half = 32
    pos = np.arange(j * T, (j + 1) * T, dtype=np.float32)
    inv_freq = (np.float32(10000.0) ** (-np.arange(half, dtype=np.float32) / np.float32(half))).astype(np.float32)
    ang = (pos[:, None] * inv_freq[None, :]).astype(np.float32)
    c = np.cos(ang).astype(np.float32).T
    s = np.sin(ang).astype(np.float32).T
    cos64 = np.concatenate([c, c], 0)
    sin64 = np.concatenate([-s, s], 0)
    cos = np.concatenate([cos64, cos64 * 0.125], 0)
    sin = np.concatenate([sin64, sin64 * 0.125], 0)
    return np.ascontiguousarray(cos, np.float32), np.ascontiguousarray(sin, np.float32)


def mixer_consts(h):
    j = np.arange(128)
    tri = (j[:, None] >= j[None, :]).astype(np.float32)
    tric = 1.0 - tri
    c = np.arange(TB)
    mask = np.stack([(c[None, :] > (128 * r + j[:, None])).astype(np.float32) for r in (3, 2, 1, 0)], 1)
    lg = np.log(np.float32(1.0) - np.float32(2.0) ** np.float32(-5.0 - h)).astype(np.float32)
    idx = np.arange(128, dtype=np.float32)
    diff = idx[None, :] - idx[:, None]
    dec = np.where(diff >= 0, np.exp(np.where(diff >= 0, diff, 0.0) * lg), 0.0).astype(np.float32)
    qdec = np.tile(np.exp((idx + 1) * lg)[None, :], (64, 1)).astype(np.float32)
    kdec = np.exp((127 - idx) * lg)[:, None].astype(np.float32)
    cdv = np.full((64, 1), np.exp(128 * lg), np.float32)
    w = POOL_WINDOWS[h]
    t = np.arange(128)
    s_ = np.arange(128)
    inwin = ((s_[:, None] <= t[None, :]) & (s_[:, None] > t[None, :] - w)).astype(np.float32)
    eye = np.eye(128, dtype=np.float32)
    b0 = inwin / w - eye
    cnt = np.minimum(t + 1, w).astype(np.float32)
    b0f = inwin / cnt[None, :] - eye
    b1 = (((s_[:, None] - 128) > (t[None, :] - w))).astype(np.float32) / w
    return dict(tri=tri.astype(NPBF), tric=tric.astype(NPBF), mask=mask.reshape(128, 4 * TB).astype(NPBF),
                dec=dec, qdec=qdec, kdec=kdec, cdv=cdv, idn=np.eye(128, dtype=np.float32).astype(NPBF),
                b0=b0.astype(NPBF), b0f=b0f.astype(NPBF), b1=b1.astype(NPBF))


CONST_SPECS = dict(tri=([128, 128], BF16), tric=([128, 128], BF16), mask=([128, 4 * TB], BF16), dec=([128, 128], F32),
                   qdec=([64, 128], F32), kdec=([128, 1], F32), cdv=([64, 1], F32), idn=([128, 128], BF16),
                   b0=([128, 128], BF16), b0f=([128, 128], BF16), b1=([128, 128], BF16))


def gcol_layout(g):
    return np.ascontiguousarray(g.reshape(NK, 128).T, np.float32)


def perm_w_in(w):
    o = {"q_sb": 0, "k_sb": 256, "v_sb": 512, "q_r": 768, "k_r": 1024, "v_r": 1280, "g_r": 1792, "u_p": 2304, "gate": 2560}
    cols = []
    for h in range(4):
        cols += list(range(o["q_sb"] + 64 * h, o["q_sb"] + 64 * h + 64))
        cols += list(range(o["k_sb"] + 64 * h, o["k_sb"] + 64 * h + 64))
        cols += list(range(o["q_r"] + 64 * h, o["q_r"] + 64 * h + 64))
        cols += list(range(o["k_r"] + 64 * h, o["k_r"] + 64 * h + 64))
        for base in (o["q_r"], o["k_r"]):
            cols += list(range(base + 64 * h + 32, base + 64 * h + 64))
            cols += list(range(base + 64 * h, base + 64 * h + 32))
    cols += list(range(o["v_sb"], o["v_sb"] + 256))
    cols += list(range(o["u_p"], o["u_p"] + 256))
    cols += list(range(o["v_r"], o["v_r"] + 512))
    cols += list(range(o["g_r"], o["g_r"] + 512))
    assert len(cols) == 3072
    return np.ascontiguousarray(w[:, cols]), np.ascontiguousarray(w[:, o["gate"]:])


GROUPS = [[0, 1, 2, 3], [4, 5, 6, 7]]
CC_MAX_ELEMS = 524288


def y_chunk_rows(rows, S):
    return min(rows, max(1, CC_MAX_ELEMS // S))


def allgather(C, src, dst, r0, nrows, rkeys, wkeys):
    C.P.op("pool", lambda e: e.collective_compute("AllGather", ALU.bypass, replica_groups=GROUPS,
                                                 ins=[src.ap()[r0:r0 + nrows, :].opt()],
                                                 outs=[dst.ap()[4 * r0:4 * (r0 + nrows), :].opt()]),
           r=rkeys, w=wkeys, dma=True, cc=True)


def perm_branch_rows(w, S):
    rows = w.shape[0] // 4
    pr = y_chunk_rows(rows, S)
    idx = [r * rows + c * pr + p for c in range(rows // pr) for r in range(4) for p in range(pr)]
    return w[idx]


def gather_y(C, yb, yg, S):
    rows = yb.shape[0]
    pr = y_chunk_rows(rows, S)
    for c in range(rows // pr):
        allgather(C, yb, yg, c * pr, pr, [], [])


def build_program(S, depth, stop=99):
    T = S // 4
    nl = T // 128
    C = Ctx()
    x_in = C.din("x_in", [D, T], F32)
    x_out = C.dout("x_out", [D, T], F32)
    xs = C.dint("xs", [D, T], F32)
    cos = C.din("p_cos", [128, T], F32)
    sin = C.din("p_sin", [128, T], F32)
    scv = C.din("p_scv", [128, 1], F32)
    gfin = C.din("gfin", [128, NK], F32)
    cst = {k: C.din("c_" + k, shp, dt) for k, (shp, dt) in CONST_SPECS.items()}
    fmA = C.dint("fmA", [D, T], BF16)
    fmG = C.dint("fmG", [4 * D, T], BF16)
    tmA = C.dint("tmA", [1536, nl * 128], BF16)
    tmG = C.dint("tmG", [4 * 1536, nl * 128], BF16)
    nb = T // TB
    fmkeys = [("fmA", b) for b in range(nb)]
    yB = [C.dint("ysbB", [64, S], BF16), C.dint("yretB", [128, S], BF16), C.dint("ypoolB", [64, S], BF16)]
    yG = [C.dint("ysbG", [256, S], BF16), C.dint("yretG", [512, S], BF16), C.dint("ypoolG", [256, S], BF16)]
    yL = [C.dint("ysbL", [256, T], BF16), C.dint("yretL", [512, T], BF16), C.dint("ypoolL", [256, T], BF16)]
    cur = x_in
    for l in range(depth):
        C.dq = "sp" if l % 2 == 0 else "act"
        W = {n: C.din(f"{n}{l}", shp, F32) for n, shp in (
            ("f1g", [128, NK]), ("f1w1", [D, FF]), ("f1w3", [D, FF]), ("f1w2", [FF, D]),
            ("pg", [128, NK]), ("pwq", [D, 3072]), ("mwg", [D, 3072]), ("mwb", [D, D]), ("mwo", [D, D]),
            ("f2g", [128, NK]), ("f2w1", [D, FF]), ("f2w3", [D, FF]), ("f2w2", [FF, D]),
            ("wp", [64, 64]), ("psc", [64, 1]))}
        emit_ffn(C, T, cur, xs, W["f1g"], W["f1w1"], W["f1w3"], W["f1w2"], None)
        if stop <= 1: break
        emit_proj(C, T, xs, W["pg"], W["pwq"], cos, sin, scv, fmA, tmA)
        C.P.barrier()
        if stop <= 2: break
        for h in range(4):
            allgather(C, fmA, fmG, 2 * h * 128, 128, fmkeys, [("fmG", 2 * h)])
        for h in range(4):
            allgather(C, tmA, tmG, (h * 3) * 128, 128, ["tmA"], [("tmG", 0, h)])
        C.P.barrier()
        for h in range(4):
            allgather(C, fmA, fmG, (2 * h + 1) * 128, 128, fmkeys, [("fmG", 2 * h + 1)])
        for g in (1, 2):
            for h in range(4):
                allgather(C, tmA, tmG, (h * 3 + g) * 128, 128, ["tmA"], [("tmG", g, h)])
        if stop <= 3: break
        C.soft_reset = True
        emit_sb(C, S, fmG, tmG, cst, yB[0])
        if stop <= 4: break
        C.P.barrier()
        gather_y(C, yB[0], yG[0], S)
        C.skip_cc = True
        C.soft_reset = False
        emit_ret(C, S, fmG, tmG, cst, yB[1])
        if stop <= 5: break
        C.P.barrier(skip_cc=True)
        gather_y(C, yB[1], yG[1], S)
        emit_pool(C, S, tmG, W["wp"], W["psc"], cst, yB[2])
        C.skip_cc = False
        C.P.barrier()
        if stop <= 6: break
        gather_y(C, yB[2], yG[2], S)
        if stop <= 7: break
        emit_merge(C, T, xs, xs, W["pg"], W["mwg"], W["mwb"], W["mwo"], yG, yL)
        last = (l == depth - 1)
        emit_ffn(C, T, xs, x_out if last else xs, W["f2g"], W["f2w1"], W["f2w3"], W["f2w2"], gfin if last else None)
        cur = xs
    C.P.barrier()
    C.P.emit()
    return C


def forward(x, g_ffn1, w1_ffn1, w3_ffn1, w2_ffn1, g_mix, w_in, w_branch_sb, w_branch_ret, w_branch_pool, w_pool,
            pool_scale, w_out, g_ffn2, w1_ffn2, w3_ffn2, w2_ffn2, g_final):
    B, S, _ = x.shape
    depth = w_in.shape[0]
    assert B == 2
    T = S // 4
    f32 = lambda a: np.ascontiguousarray(np.asarray(a, np.float32))
    xf = f32(x).reshape(B * S, D)
    common = dict(p_scv=np.concatenate([np.full((64, 1), 0.125, np.float32), np.ones((64, 1), np.float32)], 0),
                  gfin=gcol_layout(f32(g_final)))
    for l in range(depth):
        wq, wg = perm_w_in(f32(w_in[l]))
        common.update({
            f"f1g{l}": gcol_layout(f32(g_ffn1[l])), f"f1w1{l}": f32(w1_ffn1[l]), f"f1w3{l}": f32(w3_ffn1[l]),
            f"f1w2{l}": f32(w2_ffn1[l]), f"pg{l}": gcol_layout(f32(g_mix[l])), f"pwq{l}": wq, f"mwg{l}": wg,
            f"mwb{l}": f32(np.concatenate([perm_branch_rows(f32(w_branch_sb[l]), S), perm_branch_rows(f32(w_branch_ret[l]), S),
                                           perm_branch_rows(f32(w_branch_pool[l]), S)], 0)),
            f"mwo{l}": f32(w_out[l]), f"f2g{l}": gcol_layout(f32(g_ffn2[l])), f"f2w1{l}": f32(w1_ffn2[l]),
            f"f2w3{l}": f32(w3_ffn2[l]), f"f2w2{l}": f32(w2_ffn2[l])})
    in_maps = []
    for c in range(NCORES):
        h = c % 4
        m = dict(common)
        m["x_in"] = np.ascontiguousarray(xf[c * T:(c + 1) * T].T)
        m["p_cos"], m["p_sin"] = rope_tables(S, T, c % 4)
        m.update({"c_" + k: v for k, v in mixer_consts(h).items()})
        for l in range(depth):
            m[f"wp{l}"] = f32(w_pool[l][h])
            m[f"psc{l}"] = f32(np.asarray(pool_scale[l], np.float32)[h * 64:(h + 1) * 64, None])
        in_maps.append(m)
    import os
    C = build_program(S, depth, int(os.environ.get("KSTOP", "99")))
    if os.environ.get("KTRACE"):
        rr = run_bass_kernel_spmd(C.nc, in_maps, core_ids=list(range(NCORES)), trace=True)
        print("KTRACE exec_time_ns", rr.exec_time_ns)
        res = rr.results
    else:
        res = run_bass_kernel_spmd(C.nc, in_maps, core_ids=list(range(NCORES))).results
    outf = np.concatenate([res[c]["x_out"].T for c in range(NCORES)], 0)
    return np.ascontiguousarray(outf.reshape(B, S, D).astype(np.float32))


def kernel(**inputs):
    inputs = {k: np.asarray(v) for k, v in inputs.items()}
    return forward(**inputs)
```
